# Optimizing a Trainium2 kernel written in Bass

```python
import jax, jax.numpy as jnp
from jax import lax
import numpy as np

D_MODEL = 2048
BATCH = 2
SEQ = 4096
DEPTH = 2
DEC_BATCH = 128
DEC_SEQ = 4
PAST_LEN = 8192
PAGE_SIZE = 128

N_ATT = (DEPTH + 1) // 2
N_REC = DEPTH // 2
EPS = 1e-6

POOL_WINDOWS = (2, 4, 8, 16)
N_POOL_GROUPS = len(POOL_WINDOWS)
D_POOL = D_MODEL // 2
POOL_GROUP = D_POOL // N_POOL_GROUPS
POOL_STATE = max(POOL_WINDOWS) - 1

N_HEADS = 16
Q_LORA = D_MODEL // 4
KV_LORA = D_MODEL // 4
NOPE_DIM = 128
ROPE_DIM = 64
V_DIM = 128
ROPE_THETA = 10000.0
MLA_SCALE = (NOPE_DIM + ROPE_DIM) ** -0.5
Q_BLOCK = 128

HG_HEADS = 16
HG_KDIM = D_MODEL // HG_HEADS
HG_VDIM = D_MODEL // HG_HEADS
HG_CHUNK = 64

D_FF = 5632
N_EXPERTS = 8
TOP_K = 2
D_FF_EXPERT = 2816

kernel_name = 'hybrid_pool_mla_hgrn2_moe_decode_step'


def rmsnorm(x, w):
    xf = x.astype(jnp.float32)
    y = xf * lax.rsqrt(jnp.mean(xf * xf, axis=-1, keepdims=True) + EPS)
    return (y * w.astype(jnp.float32)).astype(x.dtype)


def swiglu(x, w_gu, w_down):
    g, u = jnp.split(x @ w_gu, 2, axis=-1)
    return (jax.nn.silu(g) * u) @ w_down


def rope_tables(pos):
    inv = ROPE_THETA ** (-jnp.arange(0, ROPE_DIM, 2, dtype=jnp.float32) / ROPE_DIM)
    ang = pos.astype(jnp.float32)[:, None] * inv[None, :]
    return jnp.cos(ang), jnp.sin(ang)


def apply_rope(x, cos, sin):
    x1, x2 = jnp.split(x.astype(jnp.float32), 2, axis=-1)
    return jnp.concatenate([x1 * cos - x2 * sin, x2 * cos + x1 * sin], axis=-1).astype(x.dtype)


def pool_mix(u, past, pool_w, pool_scale, start):
    L = u.shape[1]
    ext = jnp.concatenate([past, u], axis=1).astype(jnp.float32)
    cs = jnp.cumsum(ext, axis=1)
    cs = jnp.concatenate([jnp.zeros_like(cs[:, :1]), cs], axis=1)
    pos = start + jnp.arange(L)
    outs = []
    for g, w in enumerate(POOL_WINDOWS):
        lo, hi = g * POOL_GROUP, (g + 1) * POOL_GROUP
        win_sum = cs[:, POOL_STATE + 1:, lo:hi] - cs[:, POOL_STATE + 1 - w:POOL_STATE + 1 - w + L, lo:hi]
        cnt = jnp.minimum(pos + 1, w).astype(jnp.float32)[None, :, None]
        d = win_sum / cnt - ext[:, POOL_STATE:, lo:hi]
        outs.append(jnp.einsum('blc,cd->bld', d, pool_w[g].astype(jnp.float32)))
    return (jnp.concatenate(outs, axis=-1) * pool_scale.astype(jnp.float32)).astype(u.dtype)


def mla_attention(q_abs, q_pe, qpos, segs):
    B, L, H, C = q_abs.shape
    qb = Q_BLOCK if L % Q_BLOCK == 0 else L
    nb = L // qb

    def blocks(t):
        return t.reshape((B, nb, qb) + t.shape[2:]).swapaxes(0, 1)

    def one_block(args):
        qa, qp, qpos_b = args
        scores = []
        for c_seg, r_seg, kpos in segs:
            s = (jnp.einsum('bqhc,bkc->bhqk', qa, c_seg)
                 + jnp.einsum('bqhr,bkr->bhqk', qp, r_seg)).astype(jnp.float32) * MLA_SCALE
            scores.append(jnp.where(kpos[None, :] <= qpos_b[:, None], s, -jnp.inf))
        p = jax.nn.softmax(jnp.concatenate(scores, axis=-1), axis=-1)
        ctx, off = 0.0, 0
        for c_seg, _, kpos in segs:
            n = kpos.shape[0]
            ctx = ctx + jnp.einsum('bhqk,bkc->bqhc', p[..., off:off + n].astype(c_seg.dtype), c_seg)
            off += n
        return ctx

    ctx = lax.map(one_block, (blocks(q_abs), blocks(q_pe), qpos.reshape(nb, qb)))
    return ctx.swapaxes(0, 1).reshape(B, L, H, C)


def gla_chunked(q, k, v, logf, S0):
    B, L, H, _ = q.shape
    C = HG_CHUNK if L % HG_CHUNK == 0 else L
    n = L // C

    def to_chunks(t):
        return t.astype(jnp.float32).reshape(B, n, C, H, -1).transpose(1, 0, 3, 2, 4)

    causal = jnp.tril(jnp.ones((C, C), dtype=bool))

    def step(S, inp):
        qc, kc, vc, gc = inp
        b = jnp.cumsum(gc, axis=2)
        o_inter = jnp.einsum('bhtk,bhkv->bhtv', qc * jnp.exp(b), S)
        diff = b[:, :, :, None, :] - b[:, :, None, :, :]
        decay = jnp.exp(jnp.where(causal[:, :, None], diff, -jnp.inf))
        A = jnp.einsum('bhtk,bhsk,bhtsk->bhts', qc, kc, decay)
        o_intra = jnp.einsum('bhts,bhsv->bhtv', A, vc)
        b_last = b[:, :, -1:, :]
        S_new = jnp.exp(b_last[:, :, 0, :])[..., None] * S + jnp.einsum(
            'bhsk,bhsv->bhkv', kc * jnp.exp(b_last - b), vc)
        return S_new, o_inter + o_intra

    S, o = lax.scan(step, S0, (to_chunks(q), to_chunks(k), to_chunks(v), to_chunks(logf)))
    return o.transpose(1, 0, 3, 2, 4).reshape(B, L, H, -1), S


def hgrn2_mix(xn, S0, lb, w_in, hg_norm, w_out):
    B, L, _ = xn.shape
    hk, hv = HG_HEADS * HG_KDIM, HG_HEADS * HG_VDIM
    q, f, i, g = jnp.split(xn @ w_in, (hk, 2 * hk, 2 * hk + hv), axis=-1)
    logf = jnp.logaddexp(jnp.log(lb), jnp.log1p(-lb) + jax.nn.log_sigmoid(f.astype(jnp.float32)))
    k = -jnp.expm1(logf)

    def heads(t):
        return t.reshape(B, L, HG_HEADS, -1)

    o, S = gla_chunked(heads(jax.nn.silu(q)), heads(k), heads(i), heads(logf), S0.astype(jnp.float32))
    o = rmsnorm(o, hg_norm) * jax.nn.silu(heads(g)).astype(jnp.float32)
    return o.reshape(B, L, hv).astype(xn.dtype) @ w_out, S


def moe_swiglu(xn, w_router, b_router, w_exp_gu, w_exp_down):
    logits = (xn @ w_router).astype(jnp.float32) + b_router.astype(jnp.float32)
    top_v, top_i = lax.top_k(logits, TOP_K)
    gates = jax.nn.softmax(top_v, axis=-1)
    comb = jnp.sum(jax.nn.one_hot(top_i, N_EXPERTS, dtype=jnp.float32) * gates[..., None], axis=-2)
    y = jnp.zeros(xn.shape, jnp.float32)
    for e in range(N_EXPERTS):
        y = y + swiglu(xn, w_exp_gu[e], w_exp_down[e]).astype(jnp.float32) * comb[..., e:e + 1]
    return y


def even_layer(h, pool_past, kv_past, start, p):
    (norm_mix, w_in, q_norm, w_q_b, kv_norm, w_kv_b, pool_w, pool_scale, w_out, norm_ffn, w_gu, w_down) = p
    B, L, _ = h.shape
    hn = rmsnorm(h, norm_mix)
    u, q_lat, kv_lat, k_raw = jnp.split(
        hn @ w_in, (D_POOL, D_POOL + Q_LORA, D_POOL + Q_LORA + KV_LORA), axis=-1)
    pool_out = pool_mix(u, pool_past, pool_w, pool_scale, start)
    pool_new = jnp.concatenate([pool_past, u], axis=1)[:, -POOL_STATE:]
    pos = start + jnp.arange(L)
    cos, sin = rope_tables(pos)
    q = (rmsnorm(q_lat, q_norm) @ w_q_b).reshape(B, L, N_HEADS, NOPE_DIM + ROPE_DIM)
    q_nope = q[..., :NOPE_DIM]
    q_pe = apply_rope(q[..., NOPE_DIM:], cos[:, None], sin[:, None])
    c_kv = rmsnorm(kv_lat, kv_norm)
    k_pe = apply_rope(k_raw, cos, sin)
    w_kv = w_kv_b.reshape(KV_LORA, N_HEADS, NOPE_DIM + V_DIM)
    w_uk, w_uv = w_kv[..., :NOPE_DIM], w_kv[..., NOPE_DIM:]
    q_abs = jnp.einsum('blhn,chn->blhc', q_nope, w_uk)
    segs = [(c_kv, k_pe, pos)]
    if kv_past is not None:
        segs = [(kv_past[0], kv_past[1], jnp.arange(start))] + segs
    ctx = mla_attention(q_abs, q_pe, pos, segs)
    attn_out = jnp.einsum('blhc,chv->blhv', ctx, w_uv).reshape(B, L, N_HEADS * V_DIM)
    mix = jnp.concatenate([pool_out, attn_out.astype(pool_out.dtype)], axis=-1) @ w_out
    h = h + mix.astype(h.dtype)
    h = h + swiglu(rmsnorm(h, norm_ffn), w_gu, w_down).astype(h.dtype)
    return h, pool_new, c_kv, k_pe


def odd_layer(h, S0, lb, p):
    (norm_mix, w_in, hg_norm, w_out, norm_ffn, w_router, b_router, w_exp_gu, w_exp_down) = p
    out, S = hgrn2_mix(rmsnorm(h, norm_mix), S0, lb, w_in, hg_norm, w_out)
    h = h + out.astype(h.dtype)
    h = h + moe_swiglu(rmsnorm(h, norm_ffn), w_router, b_router, w_exp_gu, w_exp_down).astype(h.dtype)
    return h, S.astype(h.dtype)


def setup_inputs(seed: int = 0) -> dict:
    key = jax.random.key(seed)
    ks = iter(jax.random.split(key, 40))
    f32 = jnp.float32

    def nrm(shape, scale=1.0):
        return jax.random.normal(next(ks), shape, f32) * scale

    def gain(shape, s=0.05):
        return 1.0 + nrm(shape, s)

    n_pages = PAST_LEN // PAGE_SIZE
    n_used = DEC_BATCH * n_pages
    n_phys = (n_used * 5) // 4
    page_table = jax.random.permutation(next(ks), n_phys)[:n_used].reshape(DEC_BATCH, n_pages).astype(jnp.int32)
    d_in_e = D_POOL + Q_LORA + KV_LORA + ROPE_DIM
    d_mix_e = D_POOL + N_HEADS * V_DIM
    d_in_o = 2 * HG_HEADS * HG_KDIM + 2 * HG_HEADS * HG_VDIM
    d_mix_o = HG_HEADS * HG_VDIM
    return {
        'x_prompt': nrm((BATCH, SEQ, D_MODEL)),
        'x_sample': nrm((DEC_BATCH, DEC_SEQ, D_MODEL)),
        'cache_ckv': nrm((N_ATT, n_phys, PAGE_SIZE, KV_LORA)),
        'cache_krope': nrm((N_ATT, n_phys, PAGE_SIZE, ROPE_DIM)),
        'page_table': page_table,
        'state_pool': nrm((N_ATT, DEC_BATCH, POOL_STATE, D_POOL)),
        'state_hgrn': nrm((N_REC, DEC_BATCH, HG_HEADS, HG_KDIM, HG_VDIM), 0.5),
        'norm_mix_e': gain((N_ATT, D_MODEL)),
        'w_in_e': nrm((N_ATT, D_MODEL, d_in_e), D_MODEL ** -0.5),
        'q_norm': gain((N_ATT, Q_LORA)),
        'w_q_b': nrm((N_ATT, Q_LORA, N_HEADS * (NOPE_DIM + ROPE_DIM)), Q_LORA ** -0.5),
        'kv_norm': gain((N_ATT, KV_LORA)),
        'w_kv_b': nrm((N_ATT, KV_LORA, N_HEADS * (NOPE_DIM + V_DIM)), KV_LORA ** -0.5),
        'pool_w': nrm((N_ATT, N_POOL_GROUPS, POOL_GROUP, POOL_GROUP), POOL_GROUP ** -0.5),
        'pool_scale': gain((N_ATT, D_POOL), 0.1),
        'w_out_e': nrm((N_ATT, d_mix_e, D_MODEL), d_mix_e ** -0.5),
        'norm_ffn_e': gain((N_ATT, D_MODEL)),
        'w_ffn_gu': nrm((N_ATT, D_MODEL, 2 * D_FF), D_MODEL ** -0.5),
        'w_ffn_down': nrm((N_ATT, D_FF, D_MODEL), D_FF ** -0.5),
        'norm_mix_o': gain((N_REC, D_MODEL)),
        'w_in_o': nrm((N_REC, D_MODEL, d_in_o), D_MODEL ** -0.5),
        'hg_lower_bound': nrm((DEPTH, HG_HEADS * HG_KDIM), 0.5),
        'hg_norm': gain((N_REC, HG_VDIM)),
        'w_out_o': nrm((N_REC, d_mix_o, D_MODEL), d_mix_o ** -0.5),
        'norm_ffn_o': gain((N_REC, D_MODEL)),
        'w_router': nrm((N_REC, D_MODEL, N_EXPERTS), D_MODEL ** -0.5),
        'b_router': nrm((N_REC, N_EXPERTS), 0.01),
        'w_exp_gu': nrm((N_REC, N_EXPERTS, D_MODEL, 2 * D_FF_EXPERT), D_MODEL ** -0.5),
        'w_exp_down': nrm((N_REC, N_EXPERTS, D_FF_EXPERT, D_MODEL), D_FF_EXPERT ** -0.5),
        'final_norm': gain((D_MODEL,)),
    }


def reference(x_prompt, x_sample, cache_ckv, cache_krope, page_table, state_pool, state_hgrn,
              norm_mix_e, w_in_e, q_norm, w_q_b, kv_norm, w_kv_b, pool_w, pool_scale, w_out_e,
              norm_ffn_e, w_ffn_gu, w_ffn_down,
              norm_mix_o, w_in_o, hg_lower_bound, hg_norm, w_out_o, norm_ffn_o, w_router, b_router,
              w_exp_gu, w_exp_down, final_norm):
    n_prompt = x_prompt.shape[0]
    n_dec = page_table.shape[0]
    lb_p = jax.nn.softmax(hg_lower_bound.astype(jnp.float32), axis=0)
    lower_bounds = jnp.cumsum(lb_p, axis=0) - lb_p[0]

    hp, hs = x_prompt, x_sample
    ckv_p, krope_p, pool_p, hgrn_p = [], [], [], []
    ckv_s, krope_s, pool_s, hgrn_s = [], [], [], []
    for l in range(DEPTH):
        a = l // 2
        if l % 2 == 0:
            p = (norm_mix_e[a], w_in_e[a], q_norm[a], w_q_b[a], kv_norm[a], w_kv_b[a], pool_w[a],
                 pool_scale[a], w_out_e[a], norm_ffn_e[a], w_ffn_gu[a], w_ffn_down[a])
            zero_pool = jnp.zeros((n_prompt, POOL_STATE, D_POOL), x_prompt.dtype)
            hp, pn, cn, rn = even_layer(hp, zero_pool, None, 0, p)
            ckv_p.append(cn); krope_p.append(rn); pool_p.append(pn)
            past = (cache_ckv[a, page_table].reshape(n_dec, PAST_LEN, KV_LORA),
                    cache_krope[a, page_table].reshape(n_dec, PAST_LEN, ROPE_DIM))
            hs, pn, cn, rn = even_layer(hs, state_pool[a], past, PAST_LEN, p)
            ckv_s.append(cn); krope_s.append(rn); pool_s.append(pn)
        else:
            p = (norm_mix_o[a], w_in_o[a], hg_norm[a], w_out_o[a], norm_ffn_o[a], w_router[a], b_router[a],
                 w_exp_gu[a], w_exp_down[a])
            zero_state = jnp.zeros((n_prompt, HG_HEADS, HG_KDIM, HG_VDIM), jnp.float32)
            hp, sn = odd_layer(hp, zero_state, lower_bounds[l], p)
            hgrn_p.append(sn)
            hs, sn = odd_layer(hs, state_hgrn[a], lower_bounds[l], p)
            hgrn_s.append(sn)

    y_prompt = rmsnorm(hp, final_norm)
    y_sample = rmsnorm(hs, final_norm)
    return (y_prompt, y_sample,
            jnp.stack(ckv_p), jnp.stack(krope_p), jnp.stack(pool_p), jnp.stack(hgrn_p),
            jnp.stack(ckv_s), jnp.stack(krope_s), jnp.stack(pool_s), jnp.stack(hgrn_s))
```

```python
from concourse.bass_utils import run_bass_kernel_spmd
import numpy as np
import concourse.bass as bass
import concourse.mybir as mybir
from contextlib import ExitStack

F32 = mybir.dt.float32
BF16 = mybir.dt.bfloat16
I32 = mybir.dt.int32
ALU = mybir.AluOpType
AF = mybir.ActivationFunctionType
AX = mybir.AxisListType


class Buf:
    def __init__(self, K, t, name, dma=False):
        self.K = K
        self.t = t
        self.name = name
        self.last_write = None
        self.reads = []
        self.sem = None
        self.dcount = 0
        if dma:
            self.sem, self.dcount = K.take_sem("d_" + name)
            K.phase_tags.append(self)

    def __getitem__(self, idx):
        return self.t[idx]


class Eng:
    def __init__(self, K, name, e, selfwait):
        self.K = K
        self.name = name
        self.e = e
        self.sem = K.nc_sem("e_" + name)
        self.count = 0
        self.seen = {}
        self.selfwait = selfwait

    def wait_event(self, ev):
        if ev is None:
            return
        sem, val, src = ev
        if src is self and not self.selfwait:
            return
        key = sem.name if hasattr(sem, "name") else id(sem)
        key = self.K.semkey(sem)
        if self.seen.get(key, 0) >= val:
            return
        self.e.wait_ge(sem, val)
        self.seen[key] = val


class Kern:
    def __init__(self, nc):
        self.nc = nc
        self.root = ExitStack()
        self._semkeys = {}
        self._nk = 0
        self.stack = self.root
        self.pe = Eng(self, "pe", nc.tensor, False)
        import os
        _sw = os.environ.get("KSELF", "1") == "1"
        self.dve = Eng(self, "dve", nc.vector, _sw)
        self.act = Eng(self, "act", nc.scalar, _sw)
        self.pool = Eng(self, "pool", nc.gpsimd, True)
        self.sp = Eng(self, "sp", nc.sync, False)
        self.engs = [self.pe, self.dve, self.act, self.pool, self.sp]
        self.dma_bufs = []
        self.uid = 0
        self.sem_pool = []
        self.phase_tags = []

    def semkey(self, sem):
        k = self._semkeys.get(id(sem))
        if k is None or k[0] is not sem:
            self._nk += 1
            k = (sem, self._nk)
            self._semkeys[id(sem)] = k
        return k[1]

    def nc_sem(self, name):
        return self.root.enter_context(self.nc.semaphore(name))

    def new_sem(self, name):
        self.uid += 1
        return self.root.enter_context(self.nc.semaphore(f"{name}_{self.uid}"))

    def take_sem(self, name):
        if self.sem_pool:
            return self.sem_pool.pop()
        return (self.new_sem(name), 0)

    def sb(self, name, shape, dt=F32, dma=False):
        self.uid += 1
        t = self.stack.enter_context(self.nc.sbuf_tensor(f"{name}_{self.uid}", list(shape), dt))
        b = Buf(self, t, name, dma=dma)
        if dma:
            self.dma_bufs.append(b)
        return b

    def ps(self, name, shape, dt=F32):
        self.uid += 1
        t = self.stack.enter_context(self.nc.psum_tensor(f"{name}_{self.uid}", list(shape), dt))
        return Buf(self, t, name)

    def dram(self, name, shape, dt=F32, kind="Internal"):
        t = self.nc.dram_tensor(name, list(shape), dt, kind=kind)
        b = Buf(self, t.ap(), name, dma=False)
        return b

    def _pre(self, eng, reads, writes):
        for b in reads:
            eng.wait_event(b.last_write)
        for b in writes:
            eng.wait_event(b.last_write)
            for ev in b.reads:
                eng.wait_event(ev)

    def _post(self, ev, reads, writes):
        for b in reads:
            b.reads.append(ev)
            if len(b.reads) > 24:
                d = {}
                for e in b.reads:
                    k = self.semkey(e[0])
                    if k not in d or d[k][1] < e[1]:
                        d[k] = e
                b.reads = list(d.values())
        for b in writes:
            b.last_write = ev
            b.reads = []

    def op(self, eng, fn, reads=(), writes=()):
        self._pre(eng, reads, writes)
        ins = fn(eng.e)
        eng.count += 1
        ins.then_inc(eng.sem, 1)
        ev = (eng.sem, eng.count, eng)
        self._post(ev, reads, writes)
        return ev

    def dma(self, tag, out, in_, reads=(), writes=(), q=None):
        q = q or self.sp
        self._pre(q, reads, writes)
        with self.nc.allow_non_contiguous_dma(reason="strided layouts"):
            ins = q.e.dma_start(out=out, in_=in_)
        tag.dcount += 16
        ins.then_inc(tag.sem, 16)
        ev = (tag.sem, tag.dcount, None)
        self._post(ev, reads, writes)
        return ev

    def barrier(self):
        evs = [(e.sem, e.count, e) for e in self.engs if e.count > 0]
        for b in self.dma_bufs:
            if b.dcount > 0:
                evs.append((b.sem, b.dcount, None))
        for e in self.engs:
            for ev in evs:
                if ev[2] is e:
                    continue
                e.wait_event(ev)

    def final_wait(self):
        for b in self.dma_bufs:
            if b.dcount > 0:
                self.sp.wait_event((b.sem, b.dcount, None))
        for e in self.engs:
            if e is not self.sp and e.count > 0:
                self.sp.wait_event((e.sem, e.count, e))

    class _Phase:
        def __init__(self, K, nf=5, nb=2):
            self.K = K
            self.nf, self.nb = nf, nb
        def __enter__(self):
            self.prev = self.K.stack
            self.prev_dma = list(self.K.dma_bufs)
            self.prev_tags = self.K.phase_tags
            self.K.phase_tags = []
            self.K.stack = ExitStack()
            self.K.stack.__enter__()
            self.K.mk_ps(self.nf, self.nb)
            return self
        def __exit__(self, *a):
            self.K.barrier()
            for b in self.K.phase_tags:
                self.K.sem_pool.append((b.sem, b.dcount))
            self.K.phase_tags = self.prev_tags
            self.K.stack.__exit__(*a)
            self.K.stack = self.prev
            self.K.dma_bufs = self.prev_dma

    def phase(self, nf=5, nb=2):
        return Kern._Phase(self, nf, nb)

    def mk_ps(self, nf=5, nb=2):
        self._psf = [self.ps(f"psf{i}", [128, 512], F32) for i in range(nf)]
        self._psb = [self.ps(f"psb{i}", [128, 1024], BF16) for i in range(nb)]
        self._pi = 0
        self._pbi = 0

    def psf(self):
        self._pi = (self._pi + 1) % len(self._psf)
        return self._psf[self._pi]

    def psb(self):
        self._pbi = (self._pbi + 1) % len(self._psb)
        return self._psb[self._pbi]

NCORES = 8
REPLICATE = True
NEEDED = [f"w_exp_gu_e{e}" for e in range(8)] + [f"w_exp_down_e{e}" for e in range(8)] + ["w_in_e", "w_q_b", "w_kv_b", "pool_w", "w_out_e", "w_ffn_gu", "w_ffn_down", "w_in_o", "w_out_o"]
D = 2048
DPOOL = 1024
QL = 512
KVL = 512
RD = 64
NH = 16
EPS = 1e-6
SCALE = (128 + 64) ** -0.5
NEG = -1.0e30


def _nchunks(R, C, itemsize=4, lim=96 << 20):
    per = R // NCORES
    n = 1
    while (R * C * itemsize) // n > lim or per % n != 0:
        n += 1
        if n > per:
            raise ValueError("cannot chunk")
    return n


def _gather_table(NPHYS, DFF, DFFE, NE):
    PT = 512 if NPHYS % 512 == 0 else NPHYS
    tab = {}
    for t in range(NPHYS // PT):
        tab[f"cache_ckv_t{t}"] = (PT * 128, KVL, False)
        tab[f"cache_krope_t{t}"] = (PT * 128, RD, False)
    tab.update({"w_in_e": (D, 2112, True), "w_q_b": (QL, 3072, True), "w_kv_b": (KVL, 4096, True),
                "pool_w": (1024, 256, True), "w_out_e": (3072, D, True), "w_ffn_gu": (D, 2 * DFF, True),
                "w_ffn_down": (DFF, D, True), "w_in_o": (D, 8192, True), "w_out_o": (D, D, True)})
    for e in range(NE):
        tab[f"w_exp_gu_e{e}"] = (D, 2 * DFFE, True)
        tab[f"w_exp_down_e{e}"] = (DFFE, D, True)
    return tab, PT


class _Stop(Exception):
    pass


def _stop(level):
    import os
    if os.environ.get('KUPTO') == level:
        raise _Stop()


def build(cfg):
    SEQ, NB, NP, NPHYS, DFF, DFFE, NE = (cfg[k] for k in ("SEQ", "NB", "NP", "NPHYS", "DFF", "DFFE", "NE"))
    NTP, NTS = SEQ, NB * 4
    NT = NTP + NTS
    FC, FCE = DFF // 128, DFFE // 128
    nc = bass.Bass("TRN2", target_bir_lowering=False)
    K = Kern(nc)
    pe, dve, act, pool, sp = K.pe, K.dve, K.act, K.pool, K.sp

    def ein(name, shape, dt=F32):
        return K.dram(name, shape, dt, kind="ExternalInput")

    def eout(name, shape, dt=F32):
        return K.dram(name, shape, dt, kind="ExternalOutput")

    x_p = ein("x_prompt", [SEQ, D])
    x_s = ein("x_sample", [NTS, D])
    page_table = ein("page_table", [NB, NP], I32)
    state_pool = ein("state_pool", [NB * 15, DPOOL])
    state_hgrn = ein("state_hgrn", [NB * NH * 128, 128])
    cache_ckv = ein("cache_ckv", [NPHYS * 128, KVL])
    cache_krope = ein("cache_krope", [NPHYS * 128, RD])
    small = {}
    for nm, n in (("norm_mix_e", D), ("q_norm", QL), ("kv_norm", KVL), ("pool_scale", DPOOL), ("norm_ffn_e", D),
                  ("norm_mix_o", D), ("hg_lower_bound", 2 * D), ("hg_norm", 128), ("norm_ffn_o", D),
                  ("b_router", NE), ("final_norm", D)):
        small[nm] = ein(nm, [n])
    w_router = ein("w_router", [D, NE])
    c_cos = ein("c_cos", [64, NT])
    c_sin = ein("c_sin", [64, NT])
    c_rc = ein("c_rc", [4, NT])
    c_ident = ein("c_ident", [128, 128])
    c_dmask = ein("c_dmask", [128, 128])
    c_gmask = ein("c_gmask", [32, 32])
    c_smask = ein("c_smask", [64, 4])
    c_prot = ein("c_prot", [64, 64])
    c_sel = ein("c_sel", [NE, NE * 128])

    gtab, PT = _gather_table(NPHYS, DFF, DFFE, NE)
    gtab = {nm: v for nm, v in gtab.items() if nm in NEEDED}
    gsrc, gfull, gbf = {}, {}, {}
    for nm, (R, C, isw) in gtab.items():
        if REPLICATE:
            gsrc[nm] = ein(nm, [R, C])
            gfull[nm] = gsrc[nm]
        else:
            gsrc[nm] = ein(nm, [R // NCORES, C])
            gfull[nm] = K.dram(nm + "_full", [R, C])
        if isw:
            gbf[nm] = K.dram(nm + "_bf", [R, C], BF16)

    o_yp = eout("y_prompt", [SEQ, D])
    o_ys = eout("y_sample", [NTS, D])
    o_ckvp = eout("ckv_p", [SEQ, KVL])
    o_krp = eout("krope_p", [SEQ, RD])
    o_poolp = eout("pool_p", [15, DPOOL])
    o_hgp = eout("hgrn_p", [NH * 128, 128])
    o_ckvs = eout("ckv_s", [NTS, KVL])
    o_krs = eout("krope_s", [NTS, RD])
    o_pools = eout("pool_s", [NB * 15, DPOOL])
    o_hgs = eout("hgrn_s", [NB * NH * 128, 128])

    HT = K.dram("HT", [D, NT])
    UT = K.dram("UT", [DPOOL, NT])
    QAT = K.dram("QAT", [NH * 576, NT], BF16)
    KT = K.dram("KT", [576, NT], BF16)
    VTOK = K.dram("VTOK", [NT, KVL], BF16)
    MIXT = K.dram("MIXT", [3072, NT], BF16)
    HG = K.dram("HG", [5 * D, NT])
    OT = K.dram("OT", [D, NT])

    import os as _os2
    with K.phase():
        tag = K.sb("agtag", [128, 1], dma=True)
        tag2 = K.sb("agtag2", [128, 1], dma=True)
        ccs = K.new_sem("cc")
        ncc = 0
        for nm, (R, C, isw) in gtab.items():
            if REPLICATE:
                break
            rows = R // NCORES
            bnc = K.dram(nm + "_bnc", [rows, C])
            s_, b_ = gsrc[nm].t, bnc.t
            if rows % 128 == 0:
                s_ = s_.rearrange("(p a) c -> p (a c)", p=128)
                b_ = b_.rearrange("(p a) c -> p (a c)", p=128)
            K.dma(tag, b_, s_, q=pool)
            pool.wait_event((tag.sem, tag.dcount, None))
            ins = nc.gpsimd.collective_compute(
                "AllGather", ALU.bypass, replica_groups=[list(range(NCORES))],
                ins=[bnc.t.opt()], outs=[gfull[nm].t.opt()])
            ins.then_inc(ccs)
            ncc += 1
            pool.wait_event((ccs, ncc, None))
        for nm, (R, C, isw) in gtab.items():
            if not isw or _os2.environ.get('KV_NOCAST'):
                continue
            n = R * C
            fl = gfull[nm].t.rearrange("r c -> (r c)").rearrange("(a b) -> a b", b=2048)
            bl = gbf[nm].t.rearrange("r c -> (r c)").rearrange("(a b) -> a b", b=2048)
            nr = n // 2048
            for r0 in range(0, nr, 1024):
                r1 = min(nr, r0 + 1024)
                K.dma(tag2, bl[r0:r1, :], fl[r0:r1, :], q=pool)
        K.op(pool, lambda e: e.memset(tag[:], 0.0), writes=[tag])
    W = {nm: gbf[nm].t for nm in gbf}
    for e in range(NE):
        pass

    ident = K.sb("ident", [128, 128], dma=True)
    identb = K.sb("identb", [128, 128], BF16)
    ones = K.sb("ones", [128, 128])
    onesb = K.sb("onesb", [128, 128], BF16)
    K.dma(ident, ident[:], c_ident.t, writes=[ident])
    K.op(dve, lambda e: e.tensor_copy(out=identb[:], in_=ident[:]), reads=[ident], writes=[identb])
    K.op(dve, lambda e: e.memset(ones[:], 1.0), writes=[ones])
    K.op(dve, lambda e: e.memset(onesb[:], 1.0), writes=[onesb])

    def transpose_to(dst_ap, src_ap, srcbufs, dstbuf, np_in, nf_in, bf=False, eng=None):
        pt = K.psb() if bf else K.psf()
        idt = identb if bf else ident
        K.op(pe, lambda e: e.transpose(out=pt[0:nf_in, 0:np_in], in_=src_ap, identity=idt[0:np_in, 0:np_in]),
             reads=list(srcbufs) + [idt], writes=[pt])
        en = eng or dve
        if en is act:
            K.op(act, lambda e: e.activation(out=dst_ap, in_=pt[0:nf_in, 0:np_in], func=AF.Copy), reads=[pt], writes=[dstbuf])
        else:
            K.op(en, lambda e: e.tensor_copy(out=dst_ap, in_=pt[0:nf_in, 0:np_in]), reads=[pt], writes=[dstbuf])

    def col_param(name, n):
        a = max(1, n // 128)
        b = K.sb("c_" + name, [128, a])
        tmp = K.sb("ct_" + name, [a, 128], dma=True)
        K.dma(tmp, tmp[:], small[name].t.rearrange("(a p) -> a p", p=128), writes=[tmp])
        transpose_to(b[:, 0:a], tmp[0:a, :], [tmp], b, a, 128)
        return b

    def rmsnorm_T(xT, nch, T, wcol, out, dim, sq):
        import os
        VA, VB = os.environ.get("KV_A"), os.environ.get("KV_B")
        ps = K.psf()
        for c in range(nch):
            if VA:
                K.op(dve, lambda e: e.tensor_tensor(out=sq[:, 0:T], in0=xT[:, c, 0:T], in1=xT[:, c, 0:T], op=ALU.mult), reads=[xT], writes=[sq])
            else:
                K.op(act, lambda e: e.activation(out=sq[:, 0:T], in_=xT[:, c, 0:T], func=AF.Square), reads=[xT], writes=[sq])
            if VB:
                continue
            K.op(pe, lambda e: e.matmul(ps[:, 0:T], lhsT=ones[:], rhs=sq[:, 0:T], start=(c == 0), stop=(c == nch - 1)),
                 reads=[ones, sq], writes=[ps])
        if VB:
            K.op(dve, lambda e: e.memset(sq[:, 0:T], 1.0), writes=[sq])
        else:
            K.op(dve, lambda e: e.tensor_scalar(out=sq[:, 0:T], in0=ps[:, 0:T], scalar1=1.0 / dim, scalar2=EPS, op0=ALU.mult, op1=ALU.add),
                 reads=[ps], writes=[sq])
        if not VA:
            K.op(act, lambda e: e.activation(out=sq[:, 0:T], in_=sq[:, 0:T], func=AF.Ln), reads=[sq], writes=[sq])
            K.op(act, lambda e: e.activation(out=sq[:, 0:T], in_=sq[:, 0:T], func=AF.Exp, scale=-0.5), reads=[sq], writes=[sq])
        for c in range(nch):
            K.op(dve, lambda e: e.scalar_tensor_tensor(out=out[:, c, 0:T], in0=xT[:, c, 0:T], scalar=wcol[:, c:c + 1], in1=sq[:, 0:T],
                                                       op0=ALU.mult, op1=ALU.mult), reads=[xT, wcol, sq], writes=[out])

    class WStream:
        def __init__(self, name, kc, bw):
            self.kc, self.bw = kc, bw
            self.bufs = [K.sb(f"{name}{i}", [128, kc, bw], BF16, dma=True) for i in range(2)]
            self.i = 0

        def load(self, w_ap, c0, ncols):
            b = self.bufs[self.i]
            self.i ^= 1
            K.dma(b, b[:, :, 0:ncols], w_ap[:, c0:c0 + ncols].rearrange("(a p) n -> p a n", p=128), writes=[b])
            return b

    def linear_T(xT, kc, T, w_ap, c0, ncols, ws, evac):
        nblk = (ncols + ws.bw - 1) // ws.bw
        import os
        for bi in range(nblk):
            if bi >= int(os.environ.get('KLIN', 99)):
                break
            bc0 = bi * ws.bw
            bn = min(ws.bw, ncols - bc0)
            wb = ws.load(w_ap, c0 + bc0, bn)
            for jj in range(0, bn, 128):
                wdt = min(128, bn - jj)
                ps = K.psf()
                for k in range(kc):
                    K.op(pe, lambda e: e.matmul(ps[0:wdt, 0:T], lhsT=wb[:, k, jj:jj + wdt], rhs=xT[:, k, 0:T], start=(k == 0), stop=(k == kc - 1)),
                         reads=[wb, xT], writes=[ps])
                evac((bc0 + jj) // 128, ps, wdt)

    def tiles(n0, n1, T):
        t = n0
        while t < n1:
            yield t, min(T, n1 - t)
            t += T

    def tok_tiles(T):
        yield from tiles(0, NTP, T)
        yield from tiles(NTP, NT, T)

    import os as _os
    if _os.environ.get('KUPTO') == '0':
        K.final_wait()
        return nc, K, None
    try:
      with K.phase():
          T = 256
          nme = col_param("norm_mix_e", D)
          qnw = col_param("q_norm", QL)
          kvw = col_param("kv_norm", KVL)
          _stop('1x')
          wq = K.sb("wq", [128, 4, 3072], BF16, dma=True)
          for hh in range(2):
              K.dma(wq, wq[:, :, hh * 1536:(hh + 1) * 1536], W["w_q_b"][:, hh * 1536:(hh + 1) * 1536].rearrange("(a p) n -> p a n", p=128), writes=[wq], q=pool)
          _stop('1y')
          prot = K.sb("prot", [64, 64], dma=True)
          K.dma(prot, prot[:], c_prot.t, writes=[prot])
          wukT = K.sb("wukT", [128, NH, 512], BF16)
          wkvs = K.sb("wkvs", [128, 4, 128], BF16, dma=True)
          for h in range(NH):
              K.dma(wkvs, wkvs[:], W["w_kv_b"][:, h * 256:h * 256 + 128].rearrange("(a p) n -> p a n", p=128), writes=[wkvs])
              for cc in range(4):
                  transpose_to(wukT[:, h, cc * 128:(cc + 1) * 128], wkvs[:, cc, :], [wkvs], wukT, 128, 128, bf=True)
          _stop('1a')
          ws_in = WStream("ws_in", 16, 256)
          xtok = K.sb("xtok", [128, D], dma=True)
          xT = K.sb("xT", [128, 16, T], dma=True)
          xn = K.sb("xn", [128, 16, T], BF16)
          sq = K.sb("sq", [128, 512])
          ut = K.sb("ut", [128, T], dma=True)
          qlat = K.sb("qlat", [128, 4, T])
          kvlat = K.sb("kvlat", [128, 4, T])
          qn = K.sb("qn", [128, 4, T], BF16)
          ckvT = K.sb("ckvT", [128, 4, T], dma=True)
          kraw = K.sb("kraw", [64, T])
          rot = K.sb("rot", [64, T])
          kpe = K.sb("kpe", [64, T], dma=True)
          cosb = K.sb("cosb", [64, T], dma=True)
          sinb = K.sb("sinb", [64, T], dma=True)
          qnope = K.sb("qnope", [128, T], BF16)
          qab = K.sb("qab", [128, 4, T], BF16, dma=True)
          qpe = K.sb("qpe", [64, T])
          qpeb = K.sb("qpeb", [64, T], BF16, dma=True)
          ckvTb = K.sb("ckvTb", [128, 4, T], BF16, dma=True)
          kpeb = K.sb("kpeb", [64, T], BF16, dma=True)
          vtokb = K.sb("vtokb", [128, KVL], BF16, dma=True)
          vtok = K.sb("vtok", [128, KVL], dma=True)
          ktok = K.sb("ktok", [128, RD], dma=True)

          def rope(src, dst, Tn):
              psr = K.psf()
              K.op(pe, lambda e: e.matmul(psr[0:64, 0:Tn], lhsT=prot[:], rhs=src[:, 0:Tn], start=True, stop=True), reads=[prot, src], writes=[psr])
              K.op(dve, lambda e: e.tensor_tensor(out=rot[:, 0:Tn], in0=psr[0:64, 0:Tn], in1=sinb[:, 0:Tn], op=ALU.mult), reads=[psr, sinb], writes=[rot])
              K.op(dve, lambda e: e.tensor_tensor(out=dst[:, 0:Tn], in0=src[:, 0:Tn], in1=cosb[:, 0:Tn], op=ALU.mult), reads=[src, cosb], writes=[dst])
              K.op(dve, lambda e: e.tensor_tensor(out=dst[:, 0:Tn], in0=dst[:, 0:Tn], in1=rot[:, 0:Tn], op=ALU.add), reads=[dst, rot], writes=[dst])

          for t0, Tn in tok_tiles(T):
              issamp = t0 >= NTP
              xsrc = x_s.t if issamp else x_p.t
              r0 = t0 - NTP if issamp else t0
              for s0 in range(0, Tn, 128):
                  sn = min(128, Tn - s0)
                  K.dma(xtok, xtok[0:sn, :], xsrc[r0 + s0:r0 + s0 + sn, :], writes=[xtok])
                  for c in range(16):
                      transpose_to(xT[:, c, s0:s0 + sn], xtok[0:sn, c * 128:(c + 1) * 128], [xtok], xT, sn, 128,
                                   eng=(act if c % 2 else dve))
              if not _os.environ.get("KV_C"):
                  K.dma(xT, HT.t[:, t0:t0 + Tn].rearrange("(a p) t -> p a t", p=128), xT[:, :, 0:Tn], reads=[xT])
              K.dma(cosb, cosb[:, 0:Tn], c_cos.t[:, t0:t0 + Tn], writes=[cosb])
              K.dma(sinb, sinb[:, 0:Tn], c_sin.t[:, t0:t0 + Tn], writes=[sinb])
              _stop('1b')
              rmsnorm_T(xT, 16, Tn, nme, xn, D, sq)
              _stop('1c')

              def evac_in(j, ps, wdt):
                  if j < 8:
                      K.op(act, lambda e: e.activation(out=ut[:, 0:Tn], in_=ps[:, 0:Tn], func=AF.Copy), reads=[ps], writes=[ut])
                      if not _os.environ.get('KNOUT'):
                          K.dma(ut, UT.t[j * 128:(j + 1) * 128, t0:t0 + Tn], ut[:, 0:Tn], reads=[ut])
                  elif j < 12:
                      K.op(act, lambda e: e.activation(out=qlat[:, j - 8, 0:Tn], in_=ps[:, 0:Tn], func=AF.Copy), reads=[ps], writes=[qlat])
                  elif j < 16:
                      K.op(act, lambda e: e.activation(out=kvlat[:, j - 12, 0:Tn], in_=ps[:, 0:Tn], func=AF.Copy), reads=[ps], writes=[kvlat])
                  else:
                      K.op(act, lambda e: e.activation(out=kraw[:, 0:Tn], in_=ps[0:64, 0:Tn], func=AF.Copy), reads=[ps], writes=[kraw])
              linear_T(xn, 16, Tn, W["w_in_e"], 0, 2112, ws_in, evac_in)
              _stop('1d')
              rmsnorm_T(kvlat, 4, Tn, kvw, ckvT, KVL, sq)
              K.op(pool, lambda e: e.tensor_copy(out=ckvTb[:, :, 0:Tn], in_=ckvT[:, :, 0:Tn]), reads=[ckvT], writes=[ckvTb])
              K.dma(ckvTb, KT.t[0:512, t0:t0 + Tn].rearrange("(a p) t -> p a t", p=128), ckvTb[:, :, 0:Tn], reads=[ckvTb])
              rope(kraw, kpe, Tn)
              K.op(pool, lambda e: e.tensor_copy(out=kpeb[:, 0:Tn], in_=kpe[:, 0:Tn]), reads=[kpe], writes=[kpeb])
              K.dma(kpeb, KT.t[512:576, t0:t0 + Tn], kpeb[:, 0:Tn], reads=[kpeb])
              for s0 in range(0, Tn, 128):
                  sn = min(128, Tn - s0)
                  for c in range(4):
                      transpose_to(vtok[0:sn, c * 128:(c + 1) * 128], ckvT[:, c, s0:s0 + sn], [ckvT], vtok, 128, sn)
                  transpose_to(ktok[0:sn, :], kpe[:, s0:s0 + sn], [kpe], ktok, 64, sn)
                  K.op(pool, lambda e: e.tensor_copy(out=vtokb[0:sn, :], in_=vtok[0:sn, :]), reads=[vtok], writes=[vtokb])
                  K.dma(vtokb, VTOK.t[t0 + s0:t0 + s0 + sn, :], vtokb[0:sn, :], reads=[vtokb])
                  oc, ok = (o_ckvs, o_krs) if issamp else (o_ckvp, o_krp)
                  K.dma(vtok, oc.t[r0 + s0:r0 + s0 + sn, :], vtok[0:sn, :], reads=[vtok])
                  K.dma(ktok, ok.t[r0 + s0:r0 + s0 + sn, :], ktok[0:sn, :], reads=[ktok])
              _stop('1e')
              rmsnorm_T(qlat, 4, Tn, qnw, qn, QL, sq)
              for h in range(NH):
                  ps = K.psf()
                  for k in range(4):
                      K.op(pe, lambda e: e.matmul(ps[:, 0:Tn], lhsT=wq[:, k, h * 192:h * 192 + 128], rhs=qn[:, k, 0:Tn], start=(k == 0), stop=(k == 3)),
                           reads=[wq, qn], writes=[ps])
                  K.op(act, lambda e: e.activation(out=qnope[:, 0:Tn], in_=ps[:, 0:Tn], func=AF.Copy), reads=[ps], writes=[qnope])
                  ps2 = K.psf()
                  for k in range(4):
                      K.op(pe, lambda e: e.matmul(ps2[0:64, 0:Tn], lhsT=wq[:, k, h * 192 + 128:h * 192 + 192], rhs=qn[:, k, 0:Tn], start=(k == 0), stop=(k == 3)),
                           reads=[wq, qn], writes=[ps2])
                  K.op(act, lambda e: e.activation(out=kraw[:, 0:Tn], in_=ps2[0:64, 0:Tn], func=AF.Copy), reads=[ps2], writes=[kraw])
                  rope(kraw, qpe, Tn)
                  K.op(pool, lambda e: e.tensor_copy(out=qpeb[:, 0:Tn], in_=qpe[:, 0:Tn]), reads=[qpe], writes=[qpeb])
                  K.dma(qpeb, QAT.t[h * 576 + 512:h * 576 + 576, t0:t0 + Tn], qpeb[:, 0:Tn], reads=[qpeb])
                  for cc in range(4):
                      ps3 = K.psf()
                      K.op(pe, lambda e: e.matmul(ps3[:, 0:Tn], lhsT=wukT[:, h, cc * 128:(cc + 1) * 128], rhs=qnope[:, 0:Tn], start=True, stop=True),
                           reads=[wukT, qnope], writes=[ps3])
                      K.op(dve if cc % 2 else act,
                           (lambda e: e.tensor_copy(out=qab[:, cc, 0:Tn], in_=ps3[:, 0:Tn])) if cc % 2 else
                           (lambda e: e.activation(out=qab[:, cc, 0:Tn], in_=ps3[:, 0:Tn], func=AF.Copy)), reads=[ps3], writes=[qab])
                  K.dma(qab, QAT.t[h * 576:h * 576 + 512, t0:t0 + Tn].rearrange("(a p) t -> p a t", p=128), qab[:, :, 0:Tn], reads=[qab])
    except _Stop:
        pass

    with K.phase():
        upl = K.sb("upl", [128, 8, 16], dma=True)
        ptok = K.sb("ptok", [16, DPOOL], dma=True)
        K.dma(upl, upl[:, :, 0:15], UT.t[:, NTP - 15:NTP].rearrange("(a p) t -> p a t", p=128), writes=[upl])
        for c in range(8):
            transpose_to(ptok[0:15, c * 128:(c + 1) * 128], upl[:, c, 0:15], [upl], ptok, 128, 15)
        K.dma(ptok, o_poolp.t, ptok[0:15, :], reads=[ptok])
        ups = K.sb("ups", [128, 8, 128], dma=True)
        stok = K.sb("stok", [128, DPOOL], dma=True)
        cp = K.sb("cptag", [128, 1], dma=True)
        K.dma(cp, o_pools.t.rearrange("(b r) c -> b r c", r=15)[:, 0:11, :],
              state_pool.t.rearrange("(b r) c -> b r c", r=15)[:, 4:15, :])
        for s0 in range(0, NTS, 128):
            sn = min(128, NTS - s0)
            K.dma(ups, ups[:, :, 0:sn], UT.t[:, NTP + s0:NTP + s0 + sn].rearrange("(a p) t -> p a t", p=128), writes=[ups])
            for c in range(8):
                transpose_to(stok[0:sn, c * 128:(c + 1) * 128], ups[:, c, 0:sn], [ups], stok, 128, sn)
            for bb in range(sn // 4):
                b = s0 // 4 + bb
                K.dma(stok, o_pools.t[b * 15 + 11:b * 15 + 15, :], stok[bb * 4:bb * 4 + 4, :], reads=[stok])

    with K.phase():
        T = 256
        psc = col_param("pool_scale", DPOOL)
        pw = K.sb("pw", [128, 8, 256], BF16, dma=True)
        K.dma(pw, pw[:], W["pool_w"].rearrange("(a p) n -> p a n", p=128), writes=[pw])
        ext = K.sb("ext", [128, 8, 15 + T], dma=True)
        tA = K.sb("tA", [128, 8, 15 + T])
        tB = K.sb("tB", [128, 8, 15 + T])
        rcb = K.sb("rcb", [128, 4, T], dma=True)
        dd = K.sb("dd", [128, 8, T], BF16)
        po = K.sb("po", [128, T], BF16, dma=True)

        def pool_matmul(Tn, col0):
            for g in range(4):
                for dc in range(2):
                    ps = K.psf()
                    for cc in range(2):
                        K.op(pe, lambda e: e.matmul(ps[:, 0:Tn], lhsT=pw[:, 2 * g + cc, dc * 128:(dc + 1) * 128], rhs=dd[:, 2 * g + cc, 0:Tn],
                                                    start=(cc == 0), stop=(cc == 1)), reads=[pw, dd], writes=[ps])
                    j = 2 * g + dc
                    K.op(dve, lambda e: e.tensor_scalar(out=po[:, 0:Tn], in0=ps[:, 0:Tn], scalar1=psc[:, j:j + 1], scalar2=None, op0=ALU.mult),
                         reads=[ps, psc], writes=[po])
                    K.dma(po, MIXT.t[j * 128:(j + 1) * 128, col0:col0 + Tn], po[:, 0:Tn], reads=[po])

        for t0, Tn in tiles(0, NTP, T):
            L = 15 + Tn
            if t0 == 0:
                K.op(dve, lambda e: e.memset(ext[:, :, 0:15], 0.0), writes=[ext])
                K.dma(ext, ext[:, :, 15:L], UT.t[:, 0:Tn].rearrange("(a p) t -> p a t", p=128), writes=[ext])
            else:
                K.dma(ext, ext[:, :, 0:L], UT.t[:, t0 - 15:t0 + Tn].rearrange("(a p) t -> p a t", p=128), writes=[ext])
            K.dma(rcb, rcb[:, :, 0:Tn], c_rc.t[:, t0:t0 + Tn].partition_broadcast(128), writes=[rcb])
            K.op(dve, lambda e: e.tensor_tensor(out=tA[:, :, 1:L], in0=ext[:, :, 1:L], in1=ext[:, :, 0:L - 1], op=ALU.add), reads=[ext], writes=[tA])
            K.op(dve, lambda e: e.tensor_tensor(out=tB[:, 2:8, 3:L], in0=tA[:, 2:8, 3:L], in1=tA[:, 2:8, 1:L - 2], op=ALU.add), reads=[tA], writes=[tB])
            K.op(dve, lambda e: e.tensor_tensor(out=tA[:, 4:8, 7:L], in0=tB[:, 4:8, 7:L], in1=tB[:, 4:8, 3:L - 4], op=ALU.add), reads=[tB], writes=[tA])
            K.op(dve, lambda e: e.tensor_tensor(out=tB[:, 6:8, 15:L], in0=tA[:, 6:8, 15:L], in1=tA[:, 6:8, 7:L - 8], op=ALU.add), reads=[tA], writes=[tB])
            for c in range(8):
                g = c // 2
                src = tA if g in (0, 2) else tB
                K.op(dve, lambda e: e.tensor_tensor(out=tA[:, c, 0:Tn] if False else src[:, c, 15:L], in0=src[:, c, 15:L], in1=rcb[:, g, 0:Tn], op=ALU.mult),
                     reads=[src, rcb], writes=[src])
                K.op(dve, lambda e: e.tensor_tensor(out=dd[:, c, 0:Tn], in0=src[:, c, 15:L], in1=ext[:, c, 15:L], op=ALU.subtract),
                     reads=[src, ext], writes=[dd])
            pool_matmul(Tn, t0)

        exs = K.sb("exs", [128, 8, NB, 19], dma=True)
        sA = K.sb("sA", [128, 8, NB, 19])
        sB = K.sb("sB", [128, 8, NB, 19])
        ptk = K.sb("ptk", [120, DPOOL], dma=True)
        pT = K.sb("pT", [128, 8, 120])
        for b0 in range(0, NB, 8):
            nb = min(8, NB - b0)
            K.dma(ptk, ptk[0:nb * 15, :], state_pool.t[b0 * 15:(b0 + nb) * 15, :], writes=[ptk])
            for c in range(8):
                transpose_to(pT[:, c, 0:nb * 15], ptk[0:nb * 15, c * 128:(c + 1) * 128], [ptk], pT, nb * 15, 128)
                K.op(dve, lambda e: e.tensor_copy(out=exs[:, c, b0:b0 + nb, 0:15], in_=pT[:, c, 0:nb * 15].rearrange("p (b r) -> p b r", r=15)),
                     reads=[pT], writes=[exs])
        for c in range(8):
            K.dma(exs, exs[:, c, :, 15:19], UT.t[c * 128:(c + 1) * 128, NTP:NT].rearrange("p (b t) -> p b t", t=4), writes=[exs])
        K.dma(rcb, rcb[:, :, 0:NTS], c_rc.t[:, NTP:NT].partition_broadcast(128), writes=[rcb])
        K.op(dve, lambda e: e.tensor_tensor(out=sA[:, :, :, 1:19], in0=exs[:, :, :, 1:19], in1=exs[:, :, :, 0:18], op=ALU.add), reads=[exs], writes=[sA])
        K.op(dve, lambda e: e.tensor_tensor(out=sB[:, 2:8, :, 3:19], in0=sA[:, 2:8, :, 3:19], in1=sA[:, 2:8, :, 1:17], op=ALU.add), reads=[sA], writes=[sB])
        K.op(dve, lambda e: e.tensor_tensor(out=sA[:, 4:8, :, 7:19], in0=sB[:, 4:8, :, 7:19], in1=sB[:, 4:8, :, 3:15], op=ALU.add), reads=[sB], writes=[sA])
        K.op(dve, lambda e: e.tensor_tensor(out=sB[:, 6:8, :, 15:19], in0=sA[:, 6:8, :, 15:19], in1=sA[:, 6:8, :, 7:11], op=ALU.add), reads=[sA], writes=[sB])
        for c in range(8):
            g = c // 2
            src = sA if g in (0, 2) else sB
            K.op(dve, lambda e: e.tensor_tensor(out=src[:, c, :, 15:19], in0=src[:, c, :, 15:19], in1=rcb[:, g, 0:NTS].rearrange("p (b t) -> p b t", t=4), op=ALU.mult),
                 reads=[src, rcb], writes=[src])
            K.op(dve, lambda e: e.tensor_tensor(out=dd[:, c, 0:NTS].rearrange("p (b t) -> p b t", t=4), in0=src[:, c, :, 15:19], in1=exs[:, c, :, 15:19], op=ALU.subtract),
                 reads=[src, exs], writes=[dd])
        pool_matmul(NTS, NTP)

    with K.phase(nf=3, nb=2):
        NKT = NTP // 128
        Kt = K.sb("Kt", [128, 5, NTP], BF16, dma=True)
        Vt = K.sb("Vt", [128, NKT, KVL], BF16, dma=True)
        wuv = K.sb("wuv", [128, 4, NH, 128], BF16, dma=True)
        dmask = K.sb("dmask", [128, 128], dma=True)
        K.dma(Kt, Kt[:, 0:4, :], KT.t[0:512, 0:NTP].rearrange("(a p) t -> p a t", p=128), writes=[Kt])
        K.dma(Kt, Kt[0:64, 4, :], KT.t[512:576, 0:NTP], writes=[Kt])
        K.dma(Vt, Vt[:], VTOK.t[0:NTP, :].rearrange("(k p) c -> p k c", p=128), writes=[Vt])
        for a_ in range(4):
            K.dma(wuv, wuv[:, a_, :, :], W["w_kv_b"][a_ * 128:(a_ + 1) * 128, :].rearrange("p (h x) -> p h x", x=256)[:, :, 128:256], writes=[wuv])
        K.dma(dmask, dmask[:], c_dmask.t, writes=[dmask])
        qT = K.sb("qT", [128, NH, 5, 128], BF16, dma=True)
        S_sb = K.sb("S_sb", [128, NTP])
        Pb = K.sb("Pb", [128, NTP], BF16)
        mx = K.sb("mx", [128, 4])
        PTs = [K.sb(f"PTs{i}", [128, 512], BF16) for i in range(2)]
        ctxb = K.sb("ctxb", [128, KVL], BF16)
        ctxT = K.sb("ctxT", [128, 4, 128], BF16)
        ao = K.sb("ao", [128, NH, 128], BF16, dma=True)
        ctxps = [K.ps(f"ctxps{i}", [128, 512]) for i in range(2)]
        QV = QAT.t.rearrange("(h r) t -> h r t", r=576)
        K.op(dve, lambda e: e.memset(ao[:], 0.0), writes=[ao])
        for s0 in range(0, NTS, 128):
            sn = min(128, NTS - s0)
            K.dma(ao, MIXT.t[1024:3072, NTP + s0:NTP + s0 + sn].rearrange("(h p) t -> p h t", p=128), ao[:, :, 0:sn], reads=[ao])
        for qb in range(NKT):
            c0, c1 = qb * 128, (qb + 1) * 128
            nk = c1
            for a_ in range(4):
                K.dma(qT, qT[:, :, a_, :], QV[:, a_ * 128:(a_ + 1) * 128, c0:c1].rearrange("h p t -> p h t"), writes=[qT])
            K.dma(qT, qT[0:64, :, 4, :], QV[:, 512:576, c0:c1].rearrange("h p t -> p h t"), writes=[qT])
            for h in range(NH):
                for kc in range(0, nk, 512):
                    w = min(512, nk - kc)
                    ps = K.psf()
                    for c in range(5):
                        pp = 128 if c < 4 else 64
                        K.op(pe, lambda e: e.matmul(ps[:, 0:w], lhsT=qT[0:pp, h, c, :], rhs=Kt[0:pp, c, kc:kc + w], start=(c == 0), stop=(c == 4)),
                             reads=[qT, Kt], writes=[ps])
                    K.op(act, lambda e: e.activation(out=S_sb[:, kc:kc + w], in_=ps[:, 0:w], func=AF.Copy, scale=SCALE), reads=[ps], writes=[S_sb])
                K.op(dve, lambda e: e.tensor_tensor(out=S_sb[:, c0:c1], in0=S_sb[:, c0:c1], in1=dmask[:], op=ALU.add), reads=[S_sb, dmask], writes=[S_sb])
                K.op(dve, lambda e: e.reduce_max(out=mx[:, 0:1], in_=S_sb[:, 0:nk], axis=AX.X), reads=[S_sb], writes=[mx])
                K.op(dve, lambda e: e.tensor_scalar(out=mx[:, 1:2], in0=mx[:, 0:1], scalar1=-1.0, scalar2=None, op0=ALU.mult), reads=[mx], writes=[mx])
                K.op(dve, lambda e: e.memset(mx[:, 2:3], 0.0), writes=[mx])
                K.op(act, lambda e: e.activation(out=Pb[:, 0:nk], in_=S_sb[:, 0:nk], func=AF.Exp, bias=mx[:, 1:2], scale=1.0, accum_out=mx[:, 2:3]),
                     reads=[S_sb, mx], writes=[Pb, mx])
                cps = ctxps[h % 2]
                for k0 in range(0, qb + 1, 4):
                    nkt = min(4, qb + 1 - k0)
                    pt = K.psb()
                    for j in range(nkt):
                        K.op(pe, lambda e: e.transpose(out=pt[:, j * 128:(j + 1) * 128], in_=Pb[:, (k0 + j) * 128:(k0 + j + 1) * 128], identity=identb[:]),
                             reads=[Pb, identb], writes=[pt])
                    pts = PTs[(k0 // 4) % 2]
                    K.op(dve, lambda e: e.tensor_copy(out=pts[:, 0:nkt * 128], in_=pt[:, 0:nkt * 128]), reads=[pt], writes=[pts])
                    for j in range(nkt):
                        kt = k0 + j
                        K.op(pe, lambda e: e.matmul(cps[:, :], lhsT=pts[:, j * 128:(j + 1) * 128], rhs=Vt[:, kt, :], start=(kt == 0), stop=(kt == qb)),
                             reads=[pts, Vt], writes=[cps])
                K.op(dve, lambda e: e.reciprocal(out=mx[:, 3:4], in_=mx[:, 2:3]), reads=[mx], writes=[mx])
                K.op(act, lambda e: e.activation(out=ctxb[:], in_=cps[:, :], func=AF.Copy, scale=mx[:, 3:4]), reads=[cps, mx], writes=[ctxb])
                pt = K.psb()
                for cc in range(4):
                    K.op(pe, lambda e: e.transpose(out=pt[:, cc * 128:(cc + 1) * 128], in_=ctxb[:, cc * 128:(cc + 1) * 128], identity=identb[:]),
                         reads=[ctxb, identb], writes=[pt])
                K.op(dve, lambda e: e.tensor_copy(out=ctxT[:].rearrange("p a q -> p (a q)"), in_=pt[:, 0:512]), reads=[pt], writes=[ctxT])
                ps = K.psf()
                for cc in range(4):
                    K.op(pe, lambda e: e.matmul(ps[:, 0:128], lhsT=wuv[:, cc, h, :], rhs=ctxT[:, cc, :], start=(cc == 0), stop=(cc == 3)),
                         reads=[wuv, ctxT], writes=[ps])
                K.op(act, lambda e: e.activation(out=ao[:, h, :], in_=ps[:, 0:128], func=AF.Copy), reads=[ps], writes=[ao])
            K.dma(ao, MIXT.t[1024:3072, c0:c1].rearrange("(h p) t -> p h t", p=128), ao[:], reads=[ao])

    with K.phase(nf=3, nb=2):
        NKS = NP * 128 + 4
        wuv = K.sb("wuv", [128, 4, NH, 128], BF16, dma=True)
        for a_ in range(4):
            K.dma(wuv, wuv[:, a_, :, :], W["w_kv_b"][a_ * 128:(a_ + 1) * 128, :].rearrange("p (h x) -> p h x", x=256)[:, :, 128:256], writes=[wuv])
        smask = K.sb("smask", [64, 4], dma=True)
        K.dma(smask, smask[:], c_smask.t, writes=[smask])
        ptb = K.sb("ptb", [128, NB * NP], I32, dma=True)
        K.dma(ptb, ptb[:], page_table.t.rearrange("b j -> (b j)").partition_broadcast(128), writes=[ptb])
        ptf = K.sb("ptf", [128, NB * NP])
        iot_i = K.sb("iot_i", [128, 1], I32)
        iot = K.sb("iot", [128, 1])
        idx = K.sb("idx", [128, NB * NP], I32)
        K.op(pool, lambda e: e.iota(iot_i[:], pattern=[[0, 1]], base=0, channel_multiplier=1), writes=[iot_i])
        K.op(dve, lambda e: e.tensor_copy(out=iot[:], in_=iot_i[:]), reads=[iot_i], writes=[iot])
        K.op(dve, lambda e: e.tensor_copy(out=ptf[:], in_=ptb[:]), reads=[ptb], writes=[ptf])
        K.op(dve, lambda e: e.tensor_scalar(out=ptf[:], in0=ptf[:], scalar1=128.0, scalar2=iot[:, 0:1], op0=ALU.mult, op1=ALU.add), reads=[ptf, iot], writes=[ptf])
        K.op(dve, lambda e: e.tensor_copy(out=idx[:], in_=ptf[:]), reads=[ptf], writes=[idx])
        kv = K.sb("kv", [128, NP, 576], BF16, dma=True)
        qTs = K.sb("qTs", [128, 5, 64], BF16, dma=True)
        KTg = K.sb("KTg", [128, 5, 512], BF16)
        KTn = K.sb("KTn", [128, 5, 4], BF16, dma=True)
        Vn = K.sb("Vn", [4, KVL], BF16, dma=True)
        S_s = K.sb("S_s", [64, NKS])
        P_s = K.sb("P_s", [64, NKS], BF16)
        mxs = K.sb("mxs", [64, 4])
        ptss = [K.sb(f"ptss{i}", [128, 4, 64], BF16) for i in range(2)]
        ptn = K.sb("ptn", [4, 64], BF16)
        ctxs = K.sb("ctxs", [64, KVL], BF16)
        ctxTs = K.sb("ctxTs", [128, 4, 64], BF16)
        aos = K.sb("aos", [128, NH, 4], BF16, dma=True)
        cps = K.ps("cps_s", [128, 512])
        QV = QAT.t.rearrange("(h r) t -> h r t", r=576)
        for b in range(NB):
            col0 = NTP + 4 * b
            for c in range(5):
                pp = 128 if c < 4 else 64
                K.dma(qTs, qTs[0:pp, c, :].rearrange("p (h t) -> p h t", t=4), QV[:, c * 128:c * 128 + pp, col0:col0 + 4].rearrange("h p t -> p h t"), writes=[qTs])
            K.dma(KTn, KTn[:, 0:4, :], KT.t[0:512, col0:col0 + 4].rearrange("(a p) t -> p a t", p=128), writes=[KTn])
            K.dma(KTn, KTn[0:64, 4, :], KT.t[512:576, col0:col0 + 4], writes=[KTn])
            K.dma(Vn, Vn[:], VTOK.t[col0:col0 + 4, :], writes=[Vn])
            for j in range(NP):
                for (dst, src) in ((kv[:, j, 0:512], cache_ckv.t), (kv[:, j, 512:576], cache_krope.t)):
                    K._pre(pool, [idx], [kv])
                    ins = nc.gpsimd.indirect_dma_start(out=dst, out_offset=None, in_=src,
                                                       in_offset=bass.IndirectOffsetOnAxis(ap=idx[:, b * NP + j:b * NP + j + 1], axis=0))
                    kv.dcount += 16
                    ins.then_inc(kv.sem, 16)
                    K._post((kv.sem, kv.dcount, None), [idx], [kv])
            for g0 in range(0, NP, 4):
                ng = min(4, NP - g0)
                for c in range(5):
                    pp = 128 if c < 4 else 64
                    pt = K.psb()
                    for jj in range(ng):
                        K.op(pe, lambda e: e.transpose(out=pt[0:pp, jj * 128:(jj + 1) * 128], in_=kv[:, g0 + jj, c * 128:c * 128 + pp], identity=identb[:]),
                             reads=[kv, identb], writes=[pt])
                    K.op(dve if c % 2 else act,
                         (lambda e: e.tensor_copy(out=KTg[0:pp, c, 0:ng * 128], in_=pt[0:pp, 0:ng * 128])) if c % 2 else
                         (lambda e: e.activation(out=KTg[0:pp, c, 0:ng * 128], in_=pt[0:pp, 0:ng * 128], func=AF.Copy)), reads=[pt], writes=[KTg])
                ps = K.psf()
                for c in range(5):
                    pp = 128 if c < 4 else 64
                    K.op(pe, lambda e: e.matmul(ps[0:64, 0:ng * 128], lhsT=qTs[0:pp, c, :], rhs=KTg[0:pp, c, 0:ng * 128], start=(c == 0), stop=(c == 4)),
                         reads=[qTs, KTg], writes=[ps])
                K.op(act, lambda e: e.activation(out=S_s[:, g0 * 128:(g0 + ng) * 128], in_=ps[0:64, 0:ng * 128], func=AF.Copy, scale=SCALE), reads=[ps], writes=[S_s])
            ps = K.psf()
            for c in range(5):
                pp = 128 if c < 4 else 64
                K.op(pe, lambda e: e.matmul(ps[0:64, 0:4], lhsT=qTs[0:pp, c, :], rhs=KTn[0:pp, c, :], start=(c == 0), stop=(c == 4)), reads=[qTs, KTn], writes=[ps])
            K.op(dve, lambda e: e.scalar_tensor_tensor(out=S_s[:, NP * 128:NKS], in0=ps[0:64, 0:4], scalar=SCALE, in1=smask[:], op0=ALU.mult, op1=ALU.add),
                 reads=[ps, smask], writes=[S_s])
            K.op(dve, lambda e: e.reduce_max(out=mxs[:, 0:1], in_=S_s[:, :], axis=AX.X), reads=[S_s], writes=[mxs])
            K.op(dve, lambda e: e.tensor_scalar(out=mxs[:, 1:2], in0=mxs[:, 0:1], scalar1=-1.0, scalar2=None, op0=ALU.mult), reads=[mxs], writes=[mxs])
            K.op(dve, lambda e: e.memset(mxs[:, 2:3], 0.0), writes=[mxs])
            K.op(act, lambda e: e.activation(out=P_s[:, :], in_=S_s[:, :], func=AF.Exp, bias=mxs[:, 1:2], scale=1.0, accum_out=mxs[:, 2:3]), reads=[S_s, mxs], writes=[P_s, mxs])
            for g0 in range(0, NP, 4):
                ng = min(4, NP - g0)
                pt = K.psb()
                for jj in range(ng):
                    K.op(pe, lambda e: e.transpose(out=pt[:, jj * 64:(jj + 1) * 64], in_=P_s[:, (g0 + jj) * 128:(g0 + jj + 1) * 128], identity=identb[0:64, 0:64]),
                         reads=[P_s, identb], writes=[pt])
                pts = ptss[(g0 // 4) % 2]
                K.op(dve, lambda e: e.tensor_copy(out=pts[:].rearrange("p a q -> p (a q)")[:, 0:ng * 64], in_=pt[:, 0:ng * 64]), reads=[pt], writes=[pts])
                for jj in range(ng):
                    K.op(pe, lambda e: e.matmul(cps[0:64, :], lhsT=pts[:, jj, :], rhs=kv[:, g0 + jj, 0:512], start=(g0 + jj == 0), stop=False), reads=[pts, kv], writes=[cps])
            pt = K.psb()
            K.op(pe, lambda e: e.transpose(out=pt[0:4, 0:64], in_=P_s[:, NP * 128:NKS], identity=identb[0:64, 0:64]), reads=[P_s, identb], writes=[pt])
            K.op(dve, lambda e: e.tensor_copy(out=ptn[:], in_=pt[0:4, 0:64]), reads=[pt], writes=[ptn])
            K.op(pe, lambda e: e.matmul(cps[0:64, :], lhsT=ptn[:], rhs=Vn[:], start=False, stop=True), reads=[ptn, Vn], writes=[cps])
            K.op(dve, lambda e: e.reciprocal(out=mxs[:, 3:4], in_=mxs[:, 2:3]), reads=[mxs], writes=[mxs])
            K.op(act, lambda e: e.activation(out=ctxs[:], in_=cps[0:64, :], func=AF.Copy, scale=mxs[:, 3:4]), reads=[cps, mxs], writes=[ctxs])
            pt = K.psb()
            for cc in range(4):
                K.op(pe, lambda e: e.transpose(out=pt[:, cc * 64:(cc + 1) * 64], in_=ctxs[:, cc * 128:(cc + 1) * 128], identity=identb[0:64, 0:64]),
                     reads=[ctxs, identb], writes=[pt])
            K.op(dve, lambda e: e.tensor_copy(out=ctxTs[:].rearrange("p a q -> p (a q)"), in_=pt[:, 0:256]), reads=[pt], writes=[ctxTs])
            ps = K.psf()
            for h in range(NH):
                for cc in range(4):
                    K.op(pe, lambda e: e.matmul(ps[:, h * 4:(h + 1) * 4], lhsT=wuv[:, cc, h, :], rhs=ctxTs[:, cc, h * 4:(h + 1) * 4], start=(cc == 0), stop=(cc == 3)),
                         reads=[wuv, ctxTs], writes=[ps])
            K.op(act, lambda e: e.activation(out=aos[:].rearrange("p h t -> p (h t)"), in_=ps[:, 0:64], func=AF.Copy), reads=[ps], writes=[aos])
            K.dma(aos, MIXT.t[1024:3072, col0:col0 + 4].rearrange("(h p) t -> p h t", p=128), aos[:], reads=[aos])

    with K.phase():
        T = 512
        ws_o = WStream("ws_o", 24, 256)
        mixs = K.sb("mixs", [128, 24, T], BF16, dma=True)
        hT = K.sb("hT", [128, 16, T], dma=True)
        for t0, Tn in tok_tiles(T):
            K.dma(mixs, mixs[:, :, 0:Tn], MIXT.t[:, t0:t0 + Tn].rearrange("(a p) t -> p a t", p=128), writes=[mixs])
            K.dma(hT, hT[:, :, 0:Tn], HT.t[:, t0:t0 + Tn].rearrange("(a p) t -> p a t", p=128), writes=[hT])

            def evac_o(j, ps, wdt):
                K.op(dve, lambda e: e.tensor_tensor(out=hT[:, j, 0:Tn], in0=hT[:, j, 0:Tn], in1=ps[:, 0:Tn], op=ALU.add), reads=[hT, ps], writes=[hT])
            linear_T(mixs, 24, Tn, W["w_out_e"], 0, D, ws_o, evac_o)
            K.dma(hT, HT.t[:, t0:t0 + Tn].rearrange("(a p) t -> p a t", p=128), hT[:, :, 0:Tn], reads=[hT])

    with K.phase():
        T = 512
        nfe = col_param("norm_ffn_e", D)
        ws_gu = WStream("ws_gu", 16, 256)
        ws_d = WStream("ws_d", FC, 256)
        hT = K.sb("hT", [128, 16, T], dma=True)
        xn = K.sb("xn", [128, 16, T], BF16)
        hid = K.sb("hid", [128, FC, T], BF16)
        sq = K.sb("sq", [128, 512])
        for t0, Tn in tok_tiles(T):
            K.dma(hT, hT[:, :, 0:Tn], HT.t[:, t0:t0 + Tn].rearrange("(a p) t -> p a t", p=128), writes=[hT])
            rmsnorm_T(hT, 16, Tn, nfe, xn, D, sq)

            def evac_g(j, ps, wdt):
                K.op(act, lambda e: e.activation(out=hid[:, j, 0:Tn], in_=ps[:, 0:Tn], func=AF.Silu), reads=[ps], writes=[hid])

            def evac_u(j, ps, wdt):
                K.op(dve, lambda e: e.tensor_tensor(out=hid[:, j, 0:Tn], in0=hid[:, j, 0:Tn], in1=ps[:, 0:Tn], op=ALU.mult), reads=[hid, ps], writes=[hid])

            def evac_d(j, ps, wdt):
                K.op(dve, lambda e: e.tensor_tensor(out=hT[:, j, 0:Tn], in0=hT[:, j, 0:Tn], in1=ps[:, 0:Tn], op=ALU.add), reads=[hT, ps], writes=[hT])
            linear_T(xn, 16, Tn, W["w_ffn_gu"], 0, DFF, ws_gu, evac_g)
            linear_T(xn, 16, Tn, W["w_ffn_gu"], DFF, DFF, ws_gu, evac_u)
            linear_T(hid, FC, Tn, W["w_ffn_down"], 0, D, ws_d, evac_d)
            K.dma(hT, HT.t[:, t0:t0 + Tn].rearrange("(a p) t -> p a t", p=128), hT[:, :, 0:Tn], reads=[hT])

    with K.phase():
        T = 512
        nmo = col_param("norm_mix_o", D)
        lbp = col_param("hg_lower_bound", 2 * D)
        lb = K.sb("lb", [128, 16])
        oml = K.sb("oml", [128, 16])
        K.op(dve, lambda e: e.tensor_tensor(out=lb[:], in0=lbp[:, 16:32], in1=lbp[:, 0:16], op=ALU.subtract), reads=[lbp], writes=[lb])
        K.op(act, lambda e: e.activation(out=lb[:], in_=lb[:], func=AF.Sigmoid), reads=[lb], writes=[lb])
        K.op(dve, lambda e: e.tensor_scalar(out=oml[:], in0=lb[:], scalar1=-1.0, scalar2=1.0, op0=ALU.mult, op1=ALU.add), reads=[lb], writes=[oml])
        ws_i = WStream("ws_i", 16, 256)
        hT = K.sb("hT", [128, 16, T], dma=True)
        xn = K.sb("xn", [128, 16, T], BF16)
        sq = K.sb("sq", [128, 512])
        stg = [K.sb(f"stg{i}", [128, T], dma=True) for i in range(4)]
        stk = K.sb("stk", [128, T], dma=True)
        cnt = [0]
        for t0, Tn in tok_tiles(T):
            K.dma(hT, hT[:, :, 0:Tn], HT.t[:, t0:t0 + Tn].rearrange("(a p) t -> p a t", p=128), writes=[hT])
            rmsnorm_T(hT, 16, Tn, nmo, xn, D, sq)

            def evac_h(j, ps, wdt):
                st = stg[cnt[0] % 4]
                cnt[0] += 1
                sec, c = j // 16, j % 16
                if sec == 0 or sec == 3:
                    K.op(act, lambda e: e.activation(out=st[:, 0:Tn], in_=ps[:, 0:Tn], func=AF.Silu), reads=[ps], writes=[st])
                    row = (0 if sec == 0 else 4) * D + c * 128
                elif sec == 2:
                    K.op(act, lambda e: e.activation(out=st[:, 0:Tn], in_=ps[:, 0:Tn], func=AF.Copy), reads=[ps], writes=[st])
                    row = 3 * D + c * 128
                else:
                    K.op(act, lambda e: e.activation(out=st[:, 0:Tn], in_=ps[:, 0:Tn], func=AF.Sigmoid), reads=[ps], writes=[st])
                    K.op(dve, lambda e: e.tensor_scalar(out=st[:, 0:Tn], in0=st[:, 0:Tn], scalar1=oml[:, c:c + 1], scalar2=lb[:, c:c + 1], op0=ALU.mult, op1=ALU.add),
                         reads=[st, oml, lb], writes=[st])
                    K.op(dve, lambda e: e.tensor_scalar(out=stk[:, 0:Tn], in0=st[:, 0:Tn], scalar1=-1.0, scalar2=1.0, op0=ALU.mult, op1=ALU.add),
                         reads=[st], writes=[stk])
                    K.dma(stk, HG.t[1 * D + c * 128:1 * D + (c + 1) * 128, t0:t0 + Tn], stk[:, 0:Tn], reads=[stk])
                    K.op(act, lambda e: e.activation(out=st[:, 0:Tn], in_=st[:, 0:Tn], func=AF.Ln), reads=[st], writes=[st])
                    row = 2 * D + c * 128
                K.dma(st, HG.t[row:row + 128, t0:t0 + Tn], st[:, 0:Tn], reads=[st])
            linear_T(xn, 16, Tn, W["w_in_o"], 0, 8192, ws_i, evac_h)

    with K.phase(nf=4, nb=1):
        CP = 32
        BLK = 256
        gmask = K.sb("gmask", [32, 32], dma=True)
        K.dma(gmask, gmask[:], c_gmask.t, writes=[gmask])
        S = K.sb("S", [128, NH, 128], dma=True)
        inb = [K.sb(f"gin{i}", [128, NH, BLK], dma=True) for i in range(4)]
        otb = K.sb("otb", [128, NH, BLK], dma=True)
        bA = K.sb("bA", [128, NH, CP])
        bB = K.sb("bB", [128, NH, CP])
        eb = K.sb("eb", [128, NH, CP])
        qe = K.sb("qe", [128, NH, CP])
        kd = K.sb("kd", [128, NH, CP])
        kl = K.sb("kl", [128, NH, CP])
        atm = K.sb("atm", [32, 32])
        vtk = K.sb("vtk", [32, 128])
        klT = K.sb("klT", [32, 128])

        def gla_chunk(c0, C):
            lf = inb[2]
            src, dst = lf, bA
            first = True
            s_ = 1
            while s_ < C:
                sa = (lambda t: t[:, :, c0:c0 + C]) if first else (lambda t: t[:, :, 0:C])
                sv = sa(src)
                off = c0 if first else 0
                K.op(dve, lambda e: e.tensor_tensor(out=dst[:, :, s_:C], in0=src[:, :, off + s_:off + C], in1=src[:, :, off:off + C - s_], op=ALU.add),
                     reads=[src], writes=[dst])
                K.op(pool, lambda e: e.tensor_copy(out=dst[:, :, 0:s_], in_=src[:, :, off:off + s_]), reads=[src], writes=[dst])
                src, dst = dst, (bB if dst is bA else bA)
                first = False
                s_ *= 2
            bb = src
            boff = c0 if first else 0
            K.op(act, lambda e: e.activation(out=eb[:, :, 0:C], in_=bb[:, :, boff:boff + C], func=AF.Exp), reads=[bb], writes=[eb])
            K.op(dve, lambda e: e.tensor_tensor(out=qe[:, :, 0:C], in0=inb[0][:, :, c0:c0 + C], in1=eb[:, :, 0:C], op=ALU.mult), reads=[inb[0], eb], writes=[qe])
            K.op(act, lambda e: e.activation(out=kd[:, :, 0:C], in_=bb[:, :, boff:boff + C], func=AF.Exp, scale=-1.0), reads=[bb], writes=[kd])
            K.op(dve, lambda e: e.tensor_tensor(out=kd[:, :, 0:C], in0=kd[:, :, 0:C], in1=inb[1][:, :, c0:c0 + C], op=ALU.mult), reads=[kd, inb[1]], writes=[kd])
            K.op(dve, lambda e: e.tensor_tensor(out=kl[:, :, 0:C], in0=kd[:, :, 0:C], in1=eb[:, :, C - 1:C].to_broadcast([128, NH, C]), op=ALU.mult),
                 reads=[kd, eb], writes=[kl])
            for h in range(NH):
                pa = K.psf()
                K.op(pe, lambda e: e.matmul(pa[0:C, 0:C], lhsT=kd[:, h, 0:C], rhs=qe[:, h, 0:C], start=True, stop=True), reads=[kd, qe], writes=[pa])
                K.op(dve, lambda e: e.tensor_tensor(out=atm[0:C, 0:C], in0=pa[0:C, 0:C], in1=gmask[0:C, 0:C], op=ALU.mult), reads=[pa, gmask], writes=[atm])
                transpose_to(vtk[0:C, :], inb[3][:, h, c0:c0 + C], [inb[3]], vtk, 128, C, eng=act)
                transpose_to(klT[0:C, :], kl[:, h, 0:C], [kl], klT, 128, C, eng=act)
                po_ = K.psf()
                K.op(pe, lambda e: e.matmul(po_[:, 0:C], lhsT=S[:, h, :], rhs=qe[:, h, 0:C], start=True, stop=False), reads=[S, qe], writes=[po_])
                K.op(pe, lambda e: e.matmul(po_[:, 0:C], lhsT=vtk[0:C, :], rhs=atm[0:C, 0:C], start=False, stop=True), reads=[vtk, atm], writes=[po_])
                K.op(act, lambda e: e.activation(out=otb[:, h, c0:c0 + C], in_=po_[:, 0:C], func=AF.Copy), reads=[po_], writes=[otb])
                pn = K.psf()
                K.op(pe, lambda e: e.matmul(pn[:, 0:128], lhsT=klT[0:C, :], rhs=vtk[0:C, :], start=True, stop=True), reads=[klT, vtk], writes=[pn])
                K.op(dve, lambda e: e.scalar_tensor_tensor(out=S[:, h, :], in0=S[:, h, :], scalar=eb[:, h, C - 1:C], in1=pn[:, 0:128], op0=ALU.mult, op1=ALU.add),
                     reads=[S, eb, pn], writes=[S])

        def load_block(t0, n):
            for i, sec in enumerate((0, 1, 2, 3)):
                K.dma(inb[i], inb[i][:, :, 0:n], HG.t[sec * D:(sec + 1) * D, t0:t0 + n].rearrange("(h p) t -> p h t", p=128), writes=[inb[i]])

        K.op(dve, lambda e: e.memset(S[:], 0.0), writes=[S])
        for t0, n in tiles(0, NTP, BLK):
            load_block(t0, n)
            for c0 in range(0, n, CP):
                gla_chunk(c0, min(CP, n - c0))
            K.dma(otb, OT.t[:, t0:t0 + n].rearrange("(h p) t -> p h t", p=128), otb[:, :, 0:n], reads=[otb])
        K.dma(S, o_hgp.t.rearrange("(h k) v -> k h v", k=128), S[:], reads=[S])
        for s0, n in tiles(NTP, NT, BLK):
            load_block(s0, n)
            for bb_ in range(n // 4):
                b = (s0 - NTP) // 4 + bb_
                K.dma(S, S[:], state_hgrn.t[b * NH * 128:(b + 1) * NH * 128, :].rearrange("(h k) v -> k h v", k=128), writes=[S])
                gla_chunk(bb_ * 4, 4)
                K.dma(S, o_hgs.t[b * NH * 128:(b + 1) * NH * 128, :].rearrange("(h k) v -> k h v", k=128), S[:], reads=[S])
            K.dma(otb, OT.t[:, s0:s0 + n].rearrange("(h p) t -> p h t", p=128), otb[:, :, 0:n], reads=[otb])

    with K.phase():
        T = 512
        hgw = col_param("hg_norm", 128)
        ws_oo = WStream("ws_oo", 16, 256)
        hT = K.sb("hT", [128, 16, T], dma=True)
        oT_ = K.sb("oT_", [128, 16, T], dma=True)
        gT = K.sb("gT", [128, 16, T], dma=True)
        xo = K.sb("xo", [128, 16, T], BF16)
        sq = K.sb("sq", [128, 512])
        for t0, Tn in tok_tiles(T):
            K.dma(hT, hT[:, :, 0:Tn], HT.t[:, t0:t0 + Tn].rearrange("(a p) t -> p a t", p=128), writes=[hT])
            K.dma(oT_, oT_[:, :, 0:Tn], OT.t[:, t0:t0 + Tn].rearrange("(a p) t -> p a t", p=128), writes=[oT_])
            K.dma(gT, gT[:, :, 0:Tn], HG.t[4 * D:5 * D, t0:t0 + Tn].rearrange("(a p) t -> p a t", p=128), writes=[gT])
            for c in range(16):
                ps = K.psf()
                K.op(act, lambda e: e.activation(out=sq[:, 0:Tn], in_=oT_[:, c, 0:Tn], func=AF.Square), reads=[oT_], writes=[sq])
                K.op(pe, lambda e: e.matmul(ps[:, 0:Tn], lhsT=ones[:], rhs=sq[:, 0:Tn], start=True, stop=True), reads=[ones, sq], writes=[ps])
                K.op(dve, lambda e: e.tensor_scalar(out=sq[:, 0:Tn], in0=ps[:, 0:Tn], scalar1=1.0 / 128, scalar2=EPS, op0=ALU.mult, op1=ALU.add), reads=[ps], writes=[sq])
                K.op(act, lambda e: e.activation(out=sq[:, 0:Tn], in_=sq[:, 0:Tn], func=AF.Ln), reads=[sq], writes=[sq])
                K.op(act, lambda e: e.activation(out=sq[:, 0:Tn], in_=sq[:, 0:Tn], func=AF.Exp, scale=-0.5), reads=[sq], writes=[sq])
                K.op(dve, lambda e: e.scalar_tensor_tensor(out=oT_[:, c, 0:Tn], in0=oT_[:, c, 0:Tn], scalar=hgw[:, 0:1], in1=sq[:, 0:Tn], op0=ALU.mult, op1=ALU.mult),
                     reads=[oT_, hgw, sq], writes=[oT_])
                K.op(dve, lambda e: e.tensor_tensor(out=xo[:, c, 0:Tn], in0=oT_[:, c, 0:Tn], in1=gT[:, c, 0:Tn], op=ALU.mult), reads=[oT_, gT], writes=[xo])

            def evac_oo(j, ps, wdt):
                K.op(dve, lambda e: e.tensor_tensor(out=hT[:, j, 0:Tn], in0=hT[:, j, 0:Tn], in1=ps[:, 0:Tn], op=ALU.add), reads=[hT, ps], writes=[hT])
            linear_T(xo, 16, Tn, W["w_out_o"], 0, D, ws_oo, evac_oo)
            K.dma(hT, HT.t[:, t0:t0 + Tn].rearrange("(a p) t -> p a t", p=128), hT[:, :, 0:Tn], reads=[hT])

    with K.phase():
        T = 512
        nfo = col_param("norm_ffn_o", D)
        wr = K.sb("wr", [128, 16, NE], dma=True)
        K.dma(wr, wr[:], w_router.t.rearrange("(a p) e -> p a e", p=128), writes=[wr])
        br = K.sb("br", [NE, 1], dma=True)
        K.dma(br, br[:], small["b_router"].t.rearrange("(p o) -> p o", o=1), writes=[br])
        sel = K.sb("sel", [NE, NE * 128], dma=True)
        K.dma(sel, sel[:], c_sel.t, writes=[sel])
        ws_gu = WStream("ws_egu", 16, 256)
        ws_d = WStream("ws_ed", FCE, 256)
        hT = K.sb("hT", [128, 16, T], dma=True)
        xnf = K.sb("xnf", [128, 16, T])
        xn = K.sb("xn", [128, 16, T], BF16)
        hid = K.sb("hid", [128, FCE, T], BF16)
        tmpb = K.sb("tmpb", [128, T], BF16)
        sq = K.sb("sq", [128, 512])
        lg = K.sb("lg", [NE, T])
        lgt = K.sb("lgt", [128, NE])
        eq1 = K.sb("eq1", [128, NE])
        eq2 = K.sb("eq2", [128, NE])
        lg2 = K.sb("lg2", [128, NE])
        comb = K.sb("comb", [128, NE])
        mm = K.sb("mm", [128, 8])
        combT = K.sb("combT", [NE, T])
        cbs = K.sb("cbs", [128, NE, T])
        for t0, Tn in tok_tiles(T):
            K.dma(hT, hT[:, :, 0:Tn], HT.t[:, t0:t0 + Tn].rearrange("(a p) t -> p a t", p=128), writes=[hT])
            rmsnorm_T(hT, 16, Tn, nfo, xnf, D, sq)
            K.op(pool, lambda e: e.tensor_copy(out=xn[:, :, 0:Tn], in_=xnf[:, :, 0:Tn]), reads=[xnf], writes=[xn])
            ps = K.psf()
            for k in range(16):
                K.op(pe, lambda e: e.matmul(ps[0:NE, 0:Tn], lhsT=wr[:, k, :], rhs=xnf[:, k, 0:Tn], start=(k == 0), stop=(k == 15)), reads=[wr, xnf], writes=[ps])
            K.op(dve, lambda e: e.tensor_scalar(out=lg[:, 0:Tn], in0=ps[0:NE, 0:Tn], scalar1=br[:, 0:1], scalar2=None, op0=ALU.add), reads=[ps, br], writes=[lg])
            for s0 in range(0, Tn, 128):
                sn = min(128, Tn - s0)
                transpose_to(lgt[0:sn, :], lg[:, s0:s0 + sn], [lg], lgt, NE, sn)
                K.op(dve, lambda e: e.reduce_max(out=mm[0:sn, 0:1], in_=lgt[0:sn, :], axis=AX.X), reads=[lgt], writes=[mm])
                K.op(dve, lambda e: e.tensor_scalar(out=eq1[0:sn, :], in0=lgt[0:sn, :], scalar1=mm[0:sn, 0:1], scalar2=None, op0=ALU.is_equal), reads=[lgt, mm], writes=[eq1])
                K.op(dve, lambda e: e.scalar_tensor_tensor(out=lg2[0:sn, :], in0=eq1[0:sn, :], scalar=NEG, in1=lgt[0:sn, :], op0=ALU.mult, op1=ALU.add),
                     reads=[eq1, lgt], writes=[lg2])
                K.op(dve, lambda e: e.reduce_max(out=mm[0:sn, 1:2], in_=lg2[0:sn, :], axis=AX.X), reads=[lg2], writes=[mm])
                K.op(dve, lambda e: e.tensor_scalar(out=eq2[0:sn, :], in0=lg2[0:sn, :], scalar1=mm[0:sn, 1:2], scalar2=None, op0=ALU.is_equal), reads=[lg2, mm], writes=[eq2])
                K.op(dve, lambda e: e.tensor_tensor(out=mm[0:sn, 2:3], in0=mm[0:sn, 1:2], in1=mm[0:sn, 0:1], op=ALU.subtract), reads=[mm], writes=[mm])
                K.op(act, lambda e: e.activation(out=mm[0:sn, 3:4], in_=mm[0:sn, 2:3], func=AF.Exp), reads=[mm], writes=[mm])
                K.op(dve, lambda e: e.tensor_scalar(out=mm[0:sn, 4:5], in0=mm[0:sn, 3:4], scalar1=1.0, scalar2=None, op0=ALU.add), reads=[mm], writes=[mm])
                K.op(dve, lambda e: e.reciprocal(out=mm[0:sn, 5:6], in_=mm[0:sn, 4:5]), reads=[mm], writes=[mm])
                K.op(dve, lambda e: e.tensor_tensor(out=mm[0:sn, 6:7], in0=mm[0:sn, 3:4], in1=mm[0:sn, 5:6], op=ALU.mult), reads=[mm], writes=[mm])
                K.op(dve, lambda e: e.tensor_scalar(out=comb[0:sn, :], in0=eq1[0:sn, :], scalar1=mm[0:sn, 5:6], scalar2=None, op0=ALU.mult), reads=[eq1, mm], writes=[comb])
                K.op(dve, lambda e: e.scalar_tensor_tensor(out=comb[0:sn, :], in0=eq2[0:sn, :], scalar=mm[0:sn, 6:7], in1=comb[0:sn, :], op0=ALU.mult, op1=ALU.add),
                     reads=[eq2, mm, comb], writes=[comb])
                transpose_to(combT[:, s0:s0 + sn], comb[0:sn, :], [comb], combT, sn, NE)
            for ex in range(NE):
                ps = K.psf()
                K.op(pe, lambda e: e.matmul(ps[:, 0:Tn], lhsT=sel[:, ex * 128:(ex + 1) * 128], rhs=combT[:, 0:Tn], start=True, stop=True), reads=[sel, combT], writes=[ps])
                K.op(act, lambda e: e.activation(out=cbs[:, ex, 0:Tn], in_=ps[:, 0:Tn], func=AF.Copy), reads=[ps], writes=[cbs])
            for ex in range(NE):
                def evac_g(j, ps, wdt):
                    K.op(act, lambda e: e.activation(out=hid[:, j, 0:Tn], in_=ps[:, 0:Tn], func=AF.Silu), reads=[ps], writes=[hid])

                def evac_u(j, ps, wdt):
                    K.op(dve, lambda e: e.tensor_tensor(out=tmpb[:, 0:Tn], in0=ps[:, 0:Tn], in1=cbs[:, ex, 0:Tn], op=ALU.mult), reads=[ps, cbs], writes=[tmpb])
                    K.op(dve, lambda e: e.tensor_tensor(out=hid[:, j, 0:Tn], in0=hid[:, j, 0:Tn], in1=tmpb[:, 0:Tn], op=ALU.mult), reads=[hid, tmpb], writes=[hid])

                def evac_d(j, ps, wdt):
                    K.op(dve, lambda e: e.tensor_tensor(out=hT[:, j, 0:Tn], in0=hT[:, j, 0:Tn], in1=ps[:, 0:Tn], op=ALU.add), reads=[hT, ps], writes=[hT])
                linear_T(xn, 16, Tn, W[f"w_exp_gu_e{ex}"], 0, DFFE, ws_gu, evac_g)
                linear_T(xn, 16, Tn, W[f"w_exp_gu_e{ex}"], DFFE, DFFE, ws_gu, evac_u)
                linear_T(hid, FCE, Tn, W[f"w_exp_down_e{ex}"], 0, D, ws_d, evac_d)
            K.dma(hT, HT.t[:, t0:t0 + Tn].rearrange("(a p) t -> p a t", p=128), hT[:, :, 0:Tn], reads=[hT])

    with K.phase():
        T = 256
        fnw = col_param("final_norm", D)
        hT = K.sb("hT", [128, 16, T], dma=True)
        yn = K.sb("yn", [128, 16, T])
        sq = K.sb("sq", [128, 512])
        ytok = K.sb("ytok", [128, D], dma=True)
        for t0, Tn in tok_tiles(T):
            issamp = t0 >= NTP
            r0 = t0 - NTP if issamp else t0
            K.dma(hT, hT[:, :, 0:Tn], HT.t[:, t0:t0 + Tn].rearrange("(a p) t -> p a t", p=128), writes=[hT])
            rmsnorm_T(hT, 16, Tn, fnw, yn, D, sq)
            for s0 in range(0, Tn, 128):
                sn = min(128, Tn - s0)
                for c in range(16):
                    transpose_to(ytok[0:sn, c * 128:(c + 1) * 128], yn[:, c, s0:s0 + sn], [yn], ytok, 128, sn, eng=(act if c % 2 else dve))
                oy = o_ys if issamp else o_yp
                K.dma(ytok, oy.t[r0 + s0:r0 + s0 + sn, :], ytok[0:sn, :], reads=[ytok])
    if cfg.get("DEBUG"):
        with K.phase():
            dbg = eout("dbg_mix", [3072, NT], BF16)
            dtag = K.sb("dtag", [128, 1], dma=True)
            K.dma(dtag, dbg.t.rearrange("(a p) t -> p a t", p=128), MIXT.t.rearrange("(a p) t -> p a t", p=128))
            dbg2 = eout("dbg_ht", [D, NT])
            K.dma(dtag, dbg2.t.rearrange("(a p) t -> p a t", p=128), HT.t.rearrange("(a p) t -> p a t", p=128))
    K.final_wait()
    return nc, K, None


def _consts(SEQ, NB, NE, PAST):
    NT = SEQ + NB * 4
    pos = np.concatenate([np.arange(SEQ), np.tile(PAST + np.arange(4), NB)]).astype(np.float32)
    inv = (np.float32(10000.0) ** (-np.arange(0, 64, 2, dtype=np.float32) / np.float32(64))).astype(np.float32)
    ang = (pos[:, None] * inv[None, :]).astype(np.float32)
    cos = np.cos(ang).astype(np.float32).T
    sin = np.sin(ang).astype(np.float32).T
    c = {}
    c["c_cos"] = np.ascontiguousarray(np.concatenate([cos, cos], 0))
    c["c_sin"] = np.ascontiguousarray(np.concatenate([sin, sin], 0))
    rc = np.zeros((4, NT), np.float32)
    for g, w in enumerate((2, 4, 8, 16)):
        rc[g] = 1.0 / np.minimum(pos + 1, w)
    c["c_rc"] = rc
    c["c_ident"] = np.eye(128, dtype=np.float32)
    p = np.arange(128)
    c["c_dmask"] = np.where(p[None, :] <= p[:, None], 0.0, NEG).astype(np.float32)
    s = np.arange(32)
    c["c_gmask"] = (s[:, None] <= s[None, :]).astype(np.float32)
    r = np.arange(64)
    c["c_smask"] = np.where(np.arange(4)[None, :] <= (r % 4)[:, None], 0.0, NEG).astype(np.float32)
    pr = np.zeros((64, 64), np.float32)
    for m in range(32):
        pr[m + 32, m] = -1.0
        pr[m, m + 32] = 1.0
    c["c_prot"] = pr
    sel = np.zeros((NE, NE * 128), np.float32)
    for e in range(NE):
        sel[e, e * 128:(e + 1) * 128] = 1.0
    c["c_sel"] = sel
    return c


def _shard_rows(a2, c):
    R, C = a2.shape
    rows = R // NCORES
    return np.ascontiguousarray(a2[c * rows:(c + 1) * rows])


_CACHE = {}
_DEBUG = False
_LAST = {}


def kernel(**inp):
    x_prompt = inp["x_prompt"]
    B, SEQ, _ = x_prompt.shape
    DB = inp["x_sample"].shape[0]
    NB = DB // NCORES
    NP = inp["page_table"].shape[1]
    NPHYS = inp["cache_ckv"].shape[1]
    DFF = inp["w_ffn_down"].shape[1]
    NE, DFFE = inp["w_exp_down"].shape[1], inp["w_exp_down"].shape[2]
    cfg = dict(SEQ=SEQ, NB=NB, NP=NP, NPHYS=NPHYS, DFF=DFF, DFFE=DFFE, NE=NE)
    if _DEBUG:
        cfg["DEBUG"] = 1
    key = tuple(sorted(cfg.items()))
    if key not in _CACHE:
        _CACHE[key] = build(cfg)
    nc = _CACHE[key][0]
    f32 = lambda a: np.ascontiguousarray(a, dtype=np.float32)
    gtab, PT = _gather_table(NPHYS, DFF, DFFE, NE)
    ck = np.ascontiguousarray(inp["cache_ckv"][0].reshape(-1, KVL), dtype=np.float32)
    kr = np.ascontiguousarray(inp["cache_krope"][0].reshape(-1, RD), dtype=np.float32)
    g2 = {"w_in_e": inp["w_in_e"][0], "w_q_b": inp["w_q_b"][0], "w_kv_b": inp["w_kv_b"][0],
          "pool_w": inp["pool_w"][0].reshape(1024, 256), "w_out_e": inp["w_out_e"][0], "w_ffn_gu": inp["w_ffn_gu"][0],
          "w_ffn_down": inp["w_ffn_down"][0], "w_in_o": inp["w_in_o"][0], "w_out_o": inp["w_out_o"][0]}
    for t in range(NPHYS // PT):
        g2[f"cache_ckv_t{t}"] = ck[t * PT * 128:(t + 1) * PT * 128]
        g2[f"cache_krope_t{t}"] = kr[t * PT * 128:(t + 1) * PT * 128]
    for e in range(NE):
        g2[f"w_exp_gu_e{e}"] = inp["w_exp_gu"][0, e]
        g2[f"w_exp_down_e{e}"] = inp["w_exp_down"][0, e]
    consts = _consts(SEQ, NB, NE, NP * 128)
    in_maps = []
    for c in range(NCORES):
        m = dict(consts)
        m["x_prompt"] = f32(x_prompt[c % B])
        m["x_sample"] = f32(inp["x_sample"][c * NB:(c + 1) * NB].reshape(NB * 4, D))
        m["page_table"] = np.ascontiguousarray(inp["page_table"][c * NB:(c + 1) * NB], dtype=np.int32)
        m["state_pool"] = f32(inp["state_pool"][0, c * NB:(c + 1) * NB].reshape(NB * 15, DPOOL))
        m["state_hgrn"] = f32(inp["state_hgrn"][0, c * NB:(c + 1) * NB].reshape(NB * NH * 128, 128))
        for nm in ("norm_mix_e", "q_norm", "kv_norm", "pool_scale", "norm_ffn_e", "norm_mix_o", "hg_norm",
                   "norm_ffn_o", "b_router"):
            m[nm] = f32(inp[nm][0])
        m["hg_lower_bound"] = f32(inp["hg_lower_bound"].reshape(-1))
        m["final_norm"] = f32(inp["final_norm"])
        m["w_router"] = f32(inp["w_router"][0])
        m["cache_ckv"] = ck
        m["cache_krope"] = kr
        for nm, a2 in g2.items():
            if nm not in NEEDED:
                continue
            m[nm] = np.ascontiguousarray(a2, dtype=np.float32) if REPLICATE else _shard_rows(a2, c)
        in_maps.append(m)
    res = run_bass_kernel_spmd(nc, in_maps, core_ids=list(range(NCORES)))
    R = res.results
    _LAST["R"] = R
    cat = lambda nm, cores: np.stack([R[c][nm] for c in cores], 0)
    y_p = cat("y_prompt", range(B))
    y_s = np.concatenate([R[c]["y_sample"].reshape(NB, 4, D) for c in range(NCORES)], 0)
    ckv_p = cat("ckv_p", range(B))[None]
    kr_p = cat("krope_p", range(B))[None]
    pool_p = cat("pool_p", range(B))[None]
    hg_p = cat("hgrn_p", range(B)).reshape(B, NH, 128, 128)[None]
    ckv_s = np.concatenate([R[c]["ckv_s"].reshape(NB, 4, KVL) for c in range(NCORES)], 0)[None]
    kr_s = np.concatenate([R[c]["krope_s"].reshape(NB, 4, RD) for c in range(NCORES)], 0)[None]
    pool_s = np.concatenate([R[c]["pool_s"].reshape(NB, 15, DPOOL) for c in range(NCORES)], 0)[None]
    hg_s = np.concatenate([R[c]["hgrn_s"].reshape(NB, NH, 128, 128) for c in range(NCORES)], 0)[None]
    return (y_p, y_s, ckv_p, kr_p, pool_p, hg_p, ckv_s, kr_s, pool_s, hg_s)
```

```python
from concourse.bass_utils import run_bass_kernel_spmd
import numpy as np
import concourse.bass as bass
import concourse.mybir as mybir
from contextlib import ExitStack

F32 = mybir.dt.float32
BF16 = mybir.dt.bfloat16
I32 = mybir.dt.int32
ALU = mybir.AluOpType
AF = mybir.ActivationFunctionType
AX = mybir.AxisListType


class Buf:
    def __init__(self, K, t, name, dma=False):
        self.K = K
        self.t = t
        self.name = name
        self.last_write = None
        self.reads = []
        self.sem = None
        self.dcount = 0
        if dma:
            self.sem, self.dcount = K.take_sem("d_" + name)
            K.phase_tags.append(self)

    def __getitem__(self, idx):
        return self.t[idx]


class Eng:
    def __init__(self, K, name, e, selfwait):
        self.K = K
        self.name = name
        self.e = e
        self.sem = K.nc_sem("e_" + name)
        self.count = 0
        self.seen = {}
        self.selfwait = selfwait

    def wait_event(self, ev):
        if ev is None:
            return
        sem, val, src = ev
        if src is self and not self.selfwait:
            return
        key = sem.name if hasattr(sem, "name") else id(sem)
        key = self.K.semkey(sem)
        if self.seen.get(key, 0) >= val:
            return
        self.e.wait_ge(sem, val)
        self.seen[key] = val


class Kern:
    def __init__(self, nc):
        self.nc = nc
        self.root = ExitStack()
        self._semkeys = {}
        self._nk = 0
        self.stack = self.root
        self.pe = Eng(self, "pe", nc.tensor, False)
        import os
        _sw = os.environ.get("KSELF", "1") == "1"
        self.dve = Eng(self, "dve", nc.vector, _sw)
        self.act = Eng(self, "act", nc.scalar, _sw)
        self.pool = Eng(self, "pool", nc.gpsimd, True)
        self.sp = Eng(self, "sp", nc.sync, False)
        self.engs = [self.pe, self.dve, self.act, self.pool, self.sp]
        self.dma_bufs = []
        self.uid = 0
        self.sem_pool = []
        self.phase_tags = []

    def semkey(self, sem):
        k = self._semkeys.get(id(sem))
        if k is None or k[0] is not sem:
            self._nk += 1
            k = (sem, self._nk)
            self._semkeys[id(sem)] = k
        return k[1]

    def nc_sem(self, name):
        return self.root.enter_context(self.nc.semaphore(name))

    def new_sem(self, name):
        self.uid += 1
        return self.root.enter_context(self.nc.semaphore(f"{name}_{self.uid}"))

    def take_sem(self, name):
        if self.sem_pool:
            return self.sem_pool.pop()
        return (self.new_sem(name), 0)

    def sb(self, name, shape, dt=F32, dma=False):
        self.uid += 1
        t = self.stack.enter_context(self.nc.sbuf_tensor(f"{name}_{self.uid}", list(shape), dt))
        b = Buf(self, t, name, dma=dma)
        if dma:
            self.dma_bufs.append(b)
        return b

    def ps(self, name, shape, dt=F32):
        self.uid += 1
        t = self.stack.enter_context(self.nc.psum_tensor(f"{name}_{self.uid}", list(shape), dt))
        return Buf(self, t, name)

    def dram(self, name, shape, dt=F32, kind="Internal"):
        t = self.nc.dram_tensor(name, list(shape), dt, kind=kind)
        b = Buf(self, t.ap(), name, dma=False)
        return b

    def _pre(self, eng, reads, writes):
        for b in reads:
            eng.wait_event(b.last_write)
        for b in writes:
            eng.wait_event(b.last_write)
            for ev in b.reads:
                eng.wait_event(ev)

    def _post(self, ev, reads, writes):
        for b in reads:
            b.reads.append(ev)
            if len(b.reads) > 24:
                d = {}
                for e in b.reads:
                    k = self.semkey(e[0])
                    if k not in d or d[k][1] < e[1]:
                        d[k] = e
                b.reads = list(d.values())
        for b in writes:
            b.last_write = ev
            b.reads = []

    def op(self, eng, fn, reads=(), writes=()):
        self._pre(eng, reads, writes)
        ins = fn(eng.e)
        eng.count += 1
        ins.then_inc(eng.sem, 1)
        ev = (eng.sem, eng.count, eng)
        self._post(ev, reads, writes)
        return ev

    def dma(self, tag, out, in_, reads=(), writes=(), q=None):
        q = q or self.sp
        self._pre(q, reads, writes)
        with self.nc.allow_non_contiguous_dma(reason="strided layouts"):
            ins = q.e.dma_start(out=out, in_=in_)
        tag.dcount += 16
        ins.then_inc(tag.sem, 16)
        ev = (tag.sem, tag.dcount, None)
        self._post(ev, reads, writes)
        return ev

    def barrier(self):
        evs = [(e.sem, e.count, e) for e in self.engs if e.count > 0]
        for b in self.dma_bufs:
            if b.dcount > 0:
                evs.append((b.sem, b.dcount, None))
        for e in self.engs:
            for ev in evs:
                if ev[2] is e:
                    continue
                e.wait_event(ev)

    def final_wait(self):
        for b in self.dma_bufs:
            if b.dcount > 0:
                self.sp.wait_event((b.sem, b.dcount, None))
        for e in self.engs:
            if e is not self.sp and e.count > 0:
                self.sp.wait_event((e.sem, e.count, e))

    class _Phase:
        def __init__(self, K, nf=5, nb=2):
            self.K = K
            self.nf, self.nb = nf, nb
        def __enter__(self):
            self.prev = self.K.stack
            self.prev_dma = list(self.K.dma_bufs)
            self.prev_tags = self.K.phase_tags
            self.K.phase_tags = []
            self.K.stack = ExitStack()
            self.K.stack.__enter__()
            self.K.mk_ps(self.nf, self.nb)
            return self
        def __exit__(self, *a):
            self.K.barrier()
            for b in self.K.phase_tags:
                self.K.sem_pool.append((b.sem, b.dcount))
            self.K.phase_tags = self.prev_tags
            self.K.stack.__exit__(*a)
            self.K.stack = self.prev
            self.K.dma_bufs = self.prev_dma

    def phase(self, nf=5, nb=2):
        return Kern._Phase(self, nf, nb)

    def mk_ps(self, nf=5, nb=2):
        self._psf = [self.ps(f"psf{i}", [128, 512], F32) for i in range(nf)]
        self._psb = [self.ps(f"psb{i}", [128, 1024], BF16) for i in range(nb)]
        self._pi = 0
        self._pbi = 0

    def psf(self):
        self._pi = (self._pi + 1) % len(self._psf)
        return self._psf[self._pi]

    def psb(self):
        self._pbi = (self._pbi + 1) % len(self._psb)
        return self._psb[self._pbi]

NCORES = 8
REPLICATE = True
NEEDED = [f"w_exp_gu_e{e}" for e in range(8)] + [f"w_exp_down_e{e}" for e in range(8)] + ["w_in_e", "w_q_b", "w_kv_b", "pool_w", "w_out_e", "w_ffn_gu", "w_ffn_down", "w_in_o", "w_out_o"]
D = 2048
DPOOL = 1024
QL = 512
KVL = 512
RD = 64
NH = 16
EPS = 1e-6
SCALE = (128 + 64) ** -0.5
NEG = -1.0e30


def _nchunks(R, C, itemsize=4, lim=96 << 20):
    per = R // NCORES
    n = 1
    while (R * C * itemsize) // n > lim or per % n != 0:
        n += 1
        if n > per:
            raise ValueError("cannot chunk")
    return n


def _gather_table(NPHYS, DFF, DFFE, NE):
    PT = 512 if NPHYS % 512 == 0 else NPHYS
    tab = {}
    for t in range(NPHYS // PT):
        tab[f"cache_ckv_t{t}"] = (PT * 128, KVL, False)
        tab[f"cache_krope_t{t}"] = (PT * 128, RD, False)
    tab.update({"w_in_e": (D, 2112, True), "w_q_b": (QL, 3072, True), "w_kv_b": (KVL, 4096, True),
                "pool_w": (1024, 256, True), "w_out_e": (3072, D, True), "w_ffn_gu": (D, 2 * DFF, True),
                "w_ffn_down": (DFF, D, True), "w_in_o": (D, 8192, True), "w_out_o": (D, D, True)})
    for e in range(NE):
        tab[f"w_exp_gu_e{e}"] = (D, 2 * DFFE, True)
        tab[f"w_exp_down_e{e}"] = (DFFE, D, True)
    return tab, PT


class _Stop(Exception):
    pass


def _stop(level):
    import os
    if os.environ.get('KUPTO') == level:
        raise _Stop()


def build(cfg):
    SEQ, NB, NP, NPHYS, DFF, DFFE, NE = (cfg[k] for k in ("SEQ", "NB", "NP", "NPHYS", "DFF", "DFFE", "NE"))
    NTP, NTS = SEQ, NB * 4
    NT = NTP + NTS
    FC, FCE = DFF // 128, DFFE // 128
    nc = bass.Bass("TRN2", target_bir_lowering=False)
    K = Kern(nc)
    pe, dve, act, pool, sp = K.pe, K.dve, K.act, K.pool, K.sp

    def ein(name, shape, dt=F32):
        return K.dram(name, shape, dt, kind="ExternalInput")

    def eout(name, shape, dt=F32):
        return K.dram(name, shape, dt, kind="ExternalOutput")

    x_p = ein("x_prompt", [SEQ, D])
    x_s = ein("x_sample", [NTS, D])
    page_table = ein("page_table", [NB, NP], I32)
    state_pool = ein("state_pool", [NB * 15, DPOOL])
    state_hgrn = ein("state_hgrn", [NB * NH * 128, 128])
    cache_ckv = ein("cache_ckv", [NPHYS * 128, KVL])
    cache_krope = ein("cache_krope", [NPHYS * 128, RD])
    small = {}
    for nm, n in (("norm_mix_e", D), ("q_norm", QL), ("kv_norm", KVL), ("pool_scale", DPOOL), ("norm_ffn_e", D),
                  ("norm_mix_o", D), ("hg_lower_bound", 2 * D), ("hg_norm", 128), ("norm_ffn_o", D),
                  ("b_router", NE), ("final_norm", D)):
        small[nm] = ein(nm, [n])
    w_router = ein("w_router", [D, NE])
    c_cos = ein("c_cos", [64, NT])
    c_sin = ein("c_sin", [64, NT])
    c_rc = ein("c_rc", [4, NT])
    c_ident = ein("c_ident", [128, 128])
    c_dmask = ein("c_dmask", [128, 128])
    c_gmask = ein("c_gmask", [32, 32])
    c_smask = ein("c_smask", [64, 4])
    c_prot = ein("c_prot", [64, 64])
    c_sel = ein("c_sel", [NE, NE * 128])

    gtab, PT = _gather_table(NPHYS, DFF, DFFE, NE)
    gtab = {nm: v for nm, v in gtab.items() if nm in NEEDED}
    gsrc, gfull, gbf = {}, {}, {}
    for nm, (R, C, isw) in gtab.items():
        if REPLICATE:
            gsrc[nm] = ein(nm, [R, C])
            gfull[nm] = gsrc[nm]
        else:
            gsrc[nm] = ein(nm, [R // NCORES, C])
            gfull[nm] = K.dram(nm + "_full", [R, C])
        if isw:
            gbf[nm] = K.dram(nm + "_bf", [R, C], BF16)

    o_yp = eout("y_prompt", [SEQ, D])
    o_ys = eout("y_sample", [NTS, D])
    o_ckvp = eout("ckv_p", [SEQ, KVL])
    o_krp = eout("krope_p", [SEQ, RD])
    o_poolp = eout("pool_p", [15, DPOOL])
    o_hgp = eout("hgrn_p", [NH * 128, 128])
    o_ckvs = eout("ckv_s", [NTS, KVL])
    o_krs = eout("krope_s", [NTS, RD])
    o_pools = eout("pool_s", [NB * 15, DPOOL])
    o_hgs = eout("hgrn_s", [NB * NH * 128, 128])

    HT = K.dram("HT", [D, NT])
    UT = K.dram("UT", [DPOOL, NT])
    QAT = K.dram("QAT", [NH * 576, NT], BF16)
    KT = K.dram("KT", [576, NT], BF16)
    VTOK = K.dram("VTOK", [NT, KVL], BF16)
    MIXT = K.dram("MIXT", [3072, NT], BF16)
    HG = K.dram("HG", [5 * D, NT])
    OT = K.dram("OT", [D, NT])

    import os as _os2
    with K.phase():
        tag = K.sb("agtag", [128, 1], dma=True)
        tag2 = K.sb("agtag2", [128, 1], dma=True)
        ccs = K.new_sem("cc")
        ncc = 0
        for nm, (R, C, isw) in gtab.items():
            if REPLICATE:
                break
            rows = R // NCORES
            bnc = K.dram(nm + "_bnc", [rows, C])
            s_, b_ = gsrc[nm].t, bnc.t
            if rows % 128 == 0:
                s_ = s_.rearrange("(p a) c -> p (a c)", p=128)
                b_ = b_.rearrange("(p a) c -> p (a c)", p=128)
            K.dma(tag, b_, s_, q=pool)
            pool.wait_event((tag.sem, tag.dcount, None))
            ins = nc.gpsimd.collective_compute(
                "AllGather", ALU.bypass, replica_groups=[list(range(NCORES))],
                ins=[bnc.t.opt()], outs=[gfull[nm].t.opt()])
            ins.then_inc(ccs)
            ncc += 1
            pool.wait_event((ccs, ncc, None))
        for nm, (R, C, isw) in gtab.items():
            if not isw or _os2.environ.get('KV_NOCAST'):
                continue
            n = R * C
            fl = gfull[nm].t.rearrange("r c -> (r c)").rearrange("(a b) -> a b", b=2048)
            bl = gbf[nm].t.rearrange("r c -> (r c)").rearrange("(a b) -> a b", b=2048)
            nr = n // 2048
            for r0 in range(0, nr, 1024):
                r1 = min(nr, r0 + 1024)
                K.dma(tag2, bl[r0:r1, :], fl[r0:r1, :], q=pool)
        K.op(pool, lambda e: e.memset(tag[:], 0.0), writes=[tag])
    W = {nm: gbf[nm].t for nm in gbf}
    for e in range(NE):
        pass

    ident = K.sb("ident", [128, 128], dma=True)
    identb = K.sb("identb", [128, 128], BF16)
    ones = K.sb("ones", [128, 128])
    onesb = K.sb("onesb", [128, 128], BF16)
    K.dma(ident, ident[:], c_ident.t, writes=[ident])
    K.op(dve, lambda e: e.tensor_copy(out=identb[:], in_=ident[:]), reads=[ident], writes=[identb])
    K.op(dve, lambda e: e.memset(ones[:], 1.0), writes=[ones])
    K.op(dve, lambda e: e.memset(onesb[:], 1.0), writes=[onesb])

    def transpose_to(dst_ap, src_ap, srcbufs, dstbuf, np_in, nf_in, bf=False, eng=None):
        pt = K.psb() if bf else K.psf()
        idt = identb if bf else ident
        K.op(pe, lambda e: e.transpose(out=pt[0:nf_in, 0:np_in], in_=src_ap, identity=idt[0:np_in, 0:np_in]),
             reads=list(srcbufs) + [idt], writes=[pt])
        en = eng or dve
        if en is act:
            K.op(act, lambda e: e.activation(out=dst_ap, in_=pt[0:nf_in, 0:np_in], func=AF.Copy), reads=[pt], writes=[dstbuf])
        else:
            K.op(en, lambda e: e.tensor_copy(out=dst_ap, in_=pt[0:nf_in, 0:np_in]), reads=[pt], writes=[dstbuf])

    def col_param(name, n):
        a = max(1, n // 128)
        b = K.sb("c_" + name, [128, a])
        tmp = K.sb("ct_" + name, [a, 128], dma=True)
        K.dma(tmp, tmp[:], small[name].t.rearrange("(a p) -> a p", p=128), writes=[tmp])
        transpose_to(b[:, 0:a], tmp[0:a, :], [tmp], b, a, 128)
        return b

    def rmsnorm_T(xT, nch, T, wcol, out, dim, sq):
        import os
        VA, VB = os.environ.get("KV_A"), os.environ.get("KV_B")
        ps = K.psf()
        for c in range(nch):
            if VA:
                K.op(dve, lambda e: e.tensor_tensor(out=sq[:, 0:T], in0=xT[:, c, 0:T], in1=xT[:, c, 0:T], op=ALU.mult), reads=[xT], writes=[sq])
            else:
                K.op(act, lambda e: e.activation(out=sq[:, 0:T], in_=xT[:, c, 0:T], func=AF.Square), reads=[xT], writes=[sq])
            if VB:
                continue
            K.op(pe, lambda e: e.matmul(ps[:, 0:T], lhsT=ones[:], rhs=sq[:, 0:T], start=(c == 0), stop=(c == nch - 1)),
                 reads=[ones, sq], writes=[ps])
        if VB:
            K.op(dve, lambda e: e.memset(sq[:, 0:T], 1.0), writes=[sq])
        else:
            K.op(dve, lambda e: e.tensor_scalar(out=sq[:, 0:T], in0=ps[:, 0:T], scalar1=1.0 / dim, scalar2=EPS, op0=ALU.mult, op1=ALU.add),
                 reads=[ps], writes=[sq])
        if not VA:
            K.op(act, lambda e: e.activation(out=sq[:, 0:T], in_=sq[:, 0:T], func=AF.Ln), reads=[sq], writes=[sq])
            K.op(act, lambda e: e.activation(out=sq[:, 0:T], in_=sq[:, 0:T], func=AF.Exp, scale=-0.5), reads=[sq], writes=[sq])
        for c in range(nch):
            K.op(dve, lambda e: e.scalar_tensor_tensor(out=out[:, c, 0:T], in0=xT[:, c, 0:T], scalar=wcol[:, c:c + 1], in1=sq[:, 0:T],
                                                       op0=ALU.mult, op1=ALU.mult), reads=[xT, wcol, sq], writes=[out])

    class WStream:
        def __init__(self, name, kc, bw):
            self.kc, self.bw = kc, bw
            self.bufs = [K.sb(f"{name}{i}", [128, kc, bw], BF16, dma=True) for i in range(2)]
            self.i = 0

        def load(self, w_ap, c0, ncols):
            b = self.bufs[self.i]
            self.i ^= 1
            K.dma(b, b[:, :, 0:ncols], w_ap[:, c0:c0 + ncols].rearrange("(a p) n -> p a n", p=128), writes=[b])
            return b

    def linear_T(xT, kc, T, w_ap, c0, ncols, ws, evac):
        nblk = (ncols + ws.bw - 1) // ws.bw
        import os
        for bi in range(nblk):
            if bi >= int(os.environ.get('KLIN', 99)):
                break
            bc0 = bi * ws.bw
            bn = min(ws.bw, ncols - bc0)
            wb = ws.load(w_ap, c0 + bc0, bn)
            for jj in range(0, bn, 128):
                wdt = min(128, bn - jj)
                ps = K.psf()
                for k in range(kc):
                    K.op(pe, lambda e: e.matmul(ps[0:wdt, 0:T], lhsT=wb[:, k, jj:jj + wdt], rhs=xT[:, k, 0:T], start=(k == 0), stop=(k == kc - 1)),
                         reads=[wb, xT], writes=[ps])
                evac((bc0 + jj) // 128, ps, wdt)

    def tiles(n0, n1, T):
        t = n0
        while t < n1:
            yield t, min(T, n1 - t)
            t += T

    def tok_tiles(T):
        yield from tiles(0, NTP, T)
        yield from tiles(NTP, NT, T)

    import os as _os
    if _os.environ.get('KUPTO') == '0':
        K.final_wait()
        return nc, K, None
    try:
      with K.phase():
          T = 256
          nme = col_param("norm_mix_e", D)
          qnw = col_param("q_norm", QL)
          kvw = col_param("kv_norm", KVL)
          _stop('1x')
          wq = K.sb("wq", [128, 4, 3072], BF16, dma=True)
          for hh in range(2):
              K.dma(wq, wq[:, :, hh * 1536:(hh + 1) * 1536], W["w_q_b"][:, hh * 1536:(hh + 1) * 1536].rearrange("(a p) n -> p a n", p=128), writes=[wq], q=pool)
          _stop('1y')
          prot = K.sb("prot", [64, 64], dma=True)
          K.dma(prot, prot[:], c_prot.t, writes=[prot])
          wukT = K.sb("wukT", [128, NH, 512], BF16)
          wkvs = K.sb("wkvs", [128, 4, 128], BF16, dma=True)
          for h in range(NH):
              K.dma(wkvs, wkvs[:], W["w_kv_b"][:, h * 256:h * 256 + 128].rearrange("(a p) n -> p a n", p=128), writes=[wkvs])
              for cc in range(4):
                  transpose_to(wukT[:, h, cc * 128:(cc + 1) * 128], wkvs[:, cc, :], [wkvs], wukT, 128, 128, bf=True)
          _stop('1a')
          ws_in = WStream("ws_in", 16, 256)
          xtok = K.sb("xtok", [128, D], dma=True)
          xT = K.sb("xT", [128, 16, T], dma=True)
          xn = K.sb("xn", [128, 16, T], BF16)
          sq = K.sb("sq", [128, 512])
          ut = K.sb("ut", [128, T], dma=True)
          qlat = K.sb("qlat", [128, 4, T])
          kvlat = K.sb("kvlat", [128, 4, T])
          qn = K.sb("qn", [128, 4, T], BF16)
          ckvT = K.sb("ckvT", [128, 4, T], dma=True)
          kraw = K.sb("kraw", [64, T])
          rot = K.sb("rot", [64, T])
          kpe = K.sb("kpe", [64, T], dma=True)
          cosb = K.sb("cosb", [64, T], dma=True)
          sinb = K.sb("sinb", [64, T], dma=True)
          qnope = K.sb("qnope", [128, T], BF16)
          qab = K.sb("qab", [128, 4, T], BF16, dma=True)
          qpe = K.sb("qpe", [64, T])
          qpeb = K.sb("qpeb", [64, T], BF16, dma=True)
          ckvTb = K.sb("ckvTb", [128, 4, T], BF16, dma=True)
          kpeb = K.sb("kpeb", [64, T], BF16, dma=True)
          vtokb = K.sb("vtokb", [128, KVL], BF16, dma=True)
          vtok = K.sb("vtok", [128, KVL], dma=True)
          ktok = K.sb("ktok", [128, RD], dma=True)

          def rope(src, dst, Tn):
              psr = K.psf()
              K.op(pe, lambda e: e.matmul(psr[0:64, 0:Tn], lhsT=prot[:], rhs=src[:, 0:Tn], start=True, stop=True), reads=[prot, src], writes=[psr])
              K.op(dve, lambda e: e.tensor_tensor(out=rot[:, 0:Tn], in0=psr[0:64, 0:Tn], in1=sinb[:, 0:Tn], op=ALU.mult), reads=[psr, sinb], writes=[rot])
              K.op(dve, lambda e: e.tensor_tensor(out=dst[:, 0:Tn], in0=src[:, 0:Tn], in1=cosb[:, 0:Tn], op=ALU.mult), reads=[src, cosb], writes=[dst])
              K.op(dve, lambda e: e.tensor_tensor(out=dst[:, 0:Tn], in0=dst[:, 0:Tn], in1=rot[:, 0:Tn], op=ALU.add), reads=[dst, rot], writes=[dst])

          for t0, Tn in tok_tiles(T):
              issamp = t0 >= NTP
              xsrc = x_s.t if issamp else x_p.t
              r0 = t0 - NTP if issamp else t0
              for s0 in range(0, Tn, 128):
                  sn = min(128, Tn - s0)
                  K.dma(xtok, xtok[0:sn, :], xsrc[r0 + s0:r0 + s0 + sn, :], writes=[xtok])
                  for c in range(16):
                      transpose_to(xT[:, c, s0:s0 + sn], xtok[0:sn, c * 128:(c + 1) * 128], [xtok], xT, sn, 128,
                                   eng=(act if c % 2 else dve))
              if not _os.environ.get("KV_C"):
                  K.dma(xT, HT.t[:, t0:t0 + Tn].rearrange("(a p) t -> p a t", p=128), xT[:, :, 0:Tn], reads=[xT])
              K.dma(cosb, cosb[:, 0:Tn], c_cos.t[:, t0:t0 + Tn], writes=[cosb])
              K.dma(sinb, sinb[:, 0:Tn], c_sin.t[:, t0:t0 + Tn], writes=[sinb])
              _stop('1b')
              rmsnorm_T(xT, 16, Tn, nme, xn, D, sq)
              _stop('1c')

              def evac_in(j, ps, wdt):
                  if j < 8:
                      K.op(act, lambda e: e.activation(out=ut[:, 0:Tn], in_=ps[:, 0:Tn], func=AF.Copy), reads=[ps], writes=[ut])
                      if not _os.environ.get('KNOUT'):
                          K.dma(ut, UT.t[j * 128:(j + 1) * 128, t0:t0 + Tn], ut[:, 0:Tn], reads=[ut])
                  elif j < 12:
                      K.op(act, lambda e: e.activation(out=qlat[:, j - 8, 0:Tn], in_=ps[:, 0:Tn], func=AF.Copy), reads=[ps], writes=[qlat])
                  elif j < 16:
                      K.op(act, lambda e: e.activation(out=kvlat[:, j - 12, 0:Tn], in_=ps[:, 0:Tn], func=AF.Copy), reads=[ps], writes=[kvlat])
                  else:
                      K.op(act, lambda e: e.activation(out=kraw[:, 0:Tn], in_=ps[0:64, 0:Tn], func=AF.Copy), reads=[ps], writes=[kraw])
              linear_T(xn, 16, Tn, W["w_in_e"], 0, 2112, ws_in, evac_in)
              _stop('1d')
              rmsnorm_T(kvlat, 4, Tn, kvw, ckvT, KVL, sq)
              K.op(pool, lambda e: e.tensor_copy(out=ckvTb[:, :, 0:Tn], in_=ckvT[:, :, 0:Tn]), reads=[ckvT], writes=[ckvTb])
              K.dma(ckvTb, KT.t[0:512, t0:t0 + Tn].rearrange("(a p) t -> p a t", p=128), ckvTb[:, :, 0:Tn], reads=[ckvTb])
              rope(kraw, kpe, Tn)
              K.op(pool, lambda e: e.tensor_copy(out=kpeb[:, 0:Tn], in_=kpe[:, 0:Tn]), reads=[kpe], writes=[kpeb])
              K.dma(kpeb, KT.t[512:576, t0:t0 + Tn], kpeb[:, 0:Tn], reads=[kpeb])
              for s0 in range(0, Tn, 128):
                  sn = min(128, Tn - s0)
                  for c in range(4):
                      transpose_to(vtok[0:sn, c * 128:(c + 1) * 128], ckvT[:, c, s0:s0 + sn], [ckvT], vtok, 128, sn)
                  transpose_to(ktok[0:sn, :], kpe[:, s0:s0 + sn], [kpe], ktok, 64, sn)
                  K.op(pool, lambda e: e.tensor_copy(out=vtokb[0:sn, :], in_=vtok[0:sn, :]), reads=[vtok], writes=[vtokb])
                  K.dma(vtokb, VTOK.t[t0 + s0:t0 + s0 + sn, :], vtokb[0:sn, :], reads=[vtokb])
                  oc, ok = (o_ckvs, o_krs) if issamp else (o_ckvp, o_krp)
                  K.dma(vtok, oc.t[r0 + s0:r0 + s0 + sn, :], vtok[0:sn, :], reads=[vtok])
                  K.dma(ktok, ok.t[r0 + s0:r0 + s0 + sn, :], ktok[0:sn, :], reads=[ktok])
              _stop('1e')
              rmsnorm_T(qlat, 4, Tn, qnw, qn, QL, sq)
              for h in range(NH):
                  ps = K.psf()
                  for k in range(4):
                      K.op(pe, lambda e: e.matmul(ps[:, 0:Tn], lhsT=wq[:, k, h * 192:h * 192 + 128], rhs=qn[:, k, 0:Tn], start=(k == 0), stop=(k == 3)),
                           reads=[wq, qn], writes=[ps])
                  K.op(act, lambda e: e.activation(out=qnope[:, 0:Tn], in_=ps[:, 0:Tn], func=AF.Copy), reads=[ps], writes=[qnope])
                  ps2 = K.psf()
                  for k in range(4):
                      K.op(pe, lambda e: e.matmul(ps2[0:64, 0:Tn], lhsT=wq[:, k, h * 192 + 128:h * 192 + 192], rhs=qn[:, k, 0:Tn], start=(k == 0), stop=(k == 3)),
                           reads=[wq, qn], writes=[ps2])
                  K.op(act, lambda e: e.activation(out=kraw[:, 0:Tn], in_=ps2[0:64, 0:Tn], func=AF.Copy), reads=[ps2], writes=[kraw])
                  rope(kraw, qpe, Tn)
                  K.op(pool, lambda e: e.tensor_copy(out=qpeb[:, 0:Tn], in_=qpe[:, 0:Tn]), reads=[qpe], writes=[qpeb])
                  K.dma(qpeb, QAT.t[h * 576 + 512:h * 576 + 576, t0:t0 + Tn], qpeb[:, 0:Tn], reads=[qpeb])
                  for cc in range(4):
                      ps3 = K.psf()
                      K.op(pe, lambda e: e.matmul(ps3[:, 0:Tn], lhsT=wukT[:, h, cc * 128:(cc + 1) * 128], rhs=qnope[:, 0:Tn], start=True, stop=True),
                           reads=[wukT, qnope], writes=[ps3])
                      K.op(dve if cc % 2 else act,
                           (lambda e: e.tensor_copy(out=qab[:, cc, 0:Tn], in_=ps3[:, 0:Tn])) if cc % 2 else
                           (lambda e: e.activation(out=qab[:, cc, 0:Tn], in_=ps3[:, 0:Tn], func=AF.Copy)), reads=[ps3], writes=[qab])
                  K.dma(qab, QAT.t[h * 576:h * 576 + 512, t0:t0 + Tn].rearrange("(a p) t -> p a t", p=128), qab[:, :, 0:Tn], reads=[qab])
    except _Stop:
        pass

    with K.phase():
        upl = K.sb("upl", [128, 8, 16], dma=True)
        ptok = K.sb("ptok", [16, DPOOL], dma=True)
        K.dma(upl, upl[:, :, 0:15], UT.t[:, NTP - 15:NTP].rearrange("(a p) t -> p a t", p=128), writes=[upl])
        for c in range(8):
            transpose_to(ptok[0:15, c * 128:(c + 1) * 128], upl[:, c, 0:15], [upl], ptok, 128, 15)
        K.dma(ptok, o_poolp.t, ptok[0:15, :], reads=[ptok])
        ups = K.sb("ups", [128, 8, 128], dma=True)
        stok = K.sb("stok", [128, DPOOL], dma=True)
        cp = K.sb("cptag", [128, 1], dma=True)
        K.dma(cp, o_pools.t.rearrange("(b r) c -> b r c", r=15)[:, 0:11, :],
              state_pool.t.rearrange("(b r) c -> b r c", r=15)[:, 4:15, :])
        for s0 in range(0, NTS, 128):
            sn = min(128, NTS - s0)
            K.dma(ups, ups[:, :, 0:sn], UT.t[:, NTP + s0:NTP + s0 + sn].rearrange("(a p) t -> p a t", p=128), writes=[ups])
            for c in range(8):
                transpose_to(stok[0:sn, c * 128:(c + 1) * 128], ups[:, c, 0:sn], [ups], stok, 128, sn)
            for bb in range(sn // 4):
                b = s0 // 4 + bb
                K.dma(stok, o_pools.t[b * 15 + 11:b * 15 + 15, :], stok[bb * 4:bb * 4 + 4, :], reads=[stok])

    with K.phase():
        T = 256
        psc = col_param("pool_scale", DPOOL)
        pw = K.sb("pw", [128, 8, 256], BF16, dma=True)
        K.dma(pw, pw[:], W["pool_w"].rearrange("(a p) n -> p a n", p=128), writes=[pw])
        ext = K.sb("ext", [128, 8, 15 + T], dma=True)
        tA = K.sb("tA", [128, 8, 15 + T])
        tB = K.sb("tB", [128, 8, 15 + T])
        rcb = K.sb("rcb", [128, 4, T], dma=True)
        dd = K.sb("dd", [128, 8, T], BF16)
        po = K.sb("po", [128, T], BF16, dma=True)

        def pool_matmul(Tn, col0):
            for g in range(4):
                for dc in range(2):
                    ps = K.psf()
                    for cc in range(2):
                        K.op(pe, lambda e: e.matmul(ps[:, 0:Tn], lhsT=pw[:, 2 * g + cc, dc * 128:(dc + 1) * 128], rhs=dd[:, 2 * g + cc, 0:Tn],
                                                    start=(cc == 0), stop=(cc == 1)), reads=[pw, dd], writes=[ps])
                    j = 2 * g + dc
                    K.op(dve, lambda e: e.tensor_scalar(out=po[:, 0:Tn], in0=ps[:, 0:Tn], scalar1=psc[:, j:j + 1], scalar2=None, op0=ALU.mult),
                         reads=[ps, psc], writes=[po])
                    K.dma(po, MIXT.t[j * 128:(j + 1) * 128, col0:col0 + Tn], po[:, 0:Tn], reads=[po])

        for t0, Tn in tiles(0, NTP, T):
            L = 15 + Tn
            if t0 == 0:
                K.op(dve, lambda e: e.memset(ext[:, :, 0:15], 0.0), writes=[ext])
                K.dma(ext, ext[:, :, 15:L], UT.t[:, 0:Tn].rearrange("(a p) t -> p a t", p=128), writes=[ext])
            else:
                K.dma(ext, ext[:, :, 0:L], UT.t[:, t0 - 15:t0 + Tn].rearrange("(a p) t -> p a t", p=128), writes=[ext])
            K.dma(rcb, rcb[:, :, 0:Tn], c_rc.t[:, t0:t0 + Tn].partition_broadcast(128), writes=[rcb])
            K.op(dve, lambda e: e.tensor_tensor(out=tA[:, :, 1:L], in0=ext[:, :, 1:L], in1=ext[:, :, 0:L - 1], op=ALU.add), reads=[ext], writes=[tA])
            K.op(dve, lambda e: e.tensor_tensor(out=tB[:, 2:8, 3:L], in0=tA[:, 2:8, 3:L], in1=tA[:, 2:8, 1:L - 2], op=ALU.add), reads=[tA], writes=[tB])
            K.op(dve, lambda e: e.tensor_tensor(out=tA[:, 4:8, 7:L], in0=tB[:, 4:8, 7:L], in1=tB[:, 4:8, 3:L - 4], op=ALU.add), reads=[tB], writes=[tA])
            K.op(dve, lambda e: e.tensor_tensor(out=tB[:, 6:8, 15:L], in0=tA[:, 6:8, 15:L], in1=tA[:, 6:8, 7:L - 8], op=ALU.add), reads=[tA], writes=[tB])
            for c in range(8):
                g = c // 2
                src = tA if g in (0, 2) else tB
                K.op(dve, lambda e: e.tensor_tensor(out=tA[:, c, 0:Tn] if False else src[:, c, 15:L], in0=src[:, c, 15:L], in1=rcb[:, g, 0:Tn], op=ALU.mult),
                     reads=[src, rcb], writes=[src])
                K.op(dve, lambda e: e.tensor_tensor(out=dd[:, c, 0:Tn], in0=src[:, c, 15:L], in1=ext[:, c, 15:L], op=ALU.subtract),
                     reads=[src, ext], writes=[dd])
            pool_matmul(Tn, t0)

        exs = K.sb("exs", [128, 8, NB, 19], dma=True)
        sA = K.sb("sA", [128, 8, NB, 19])
        sB = K.sb("sB", [128, 8, NB, 19])
        ptk = K.sb("ptk", [120, DPOOL], dma=True)
        pT = K.sb("pT", [128, 8, 120])
        for b0 in range(0, NB, 8):
            nb = min(8, NB - b0)
            K.dma(ptk, ptk[0:nb * 15, :], state_pool.t[b0 * 15:(b0 + nb) * 15, :], writes=[ptk])
            for c in range(8):
                transpose_to(pT[:, c, 0:nb * 15], ptk[0:nb * 15, c * 128:(c + 1) * 128], [ptk], pT, nb * 15, 128)
                K.op(dve, lambda e: e.tensor_copy(out=exs[:, c, b0:b0 + nb, 0:15], in_=pT[:, c, 0:nb * 15].rearrange("p (b r) -> p b r", r=15)),
                     reads=[pT], writes=[exs])
        for c in range(8):
            K.dma(exs, exs[:, c, :, 15:19], UT.t[c * 128:(c + 1) * 128, NTP:NT].rearrange("p (b t) -> p b t", t=4), writes=[exs])
        K.dma(rcb, rcb[:, :, 0:NTS], c_rc.t[:, NTP:NT].partition_broadcast(128), writes=[rcb])
        K.op(dve, lambda e: e.tensor_tensor(out=sA[:, :, :, 1:19], in0=exs[:, :, :, 1:19], in1=exs[:, :, :, 0:18], op=ALU.add), reads=[exs], writes=[sA])
        K.op(dve, lambda e: e.tensor_tensor(out=sB[:, 2:8, :, 3:19], in0=sA[:, 2:8, :, 3:19], in1=sA[:, 2:8, :, 1:17], op=ALU.add), reads=[sA], writes=[sB])
        K.op(dve, lambda e: e.tensor_tensor(out=sA[:, 4:8, :, 7:19], in0=sB[:, 4:8, :, 7:19], in1=sB[:, 4:8, :, 3:15], op=ALU.add), reads=[sB], writes=[sA])
        K.op(dve, lambda e: e.tensor_tensor(out=sB[:, 6:8, :, 15:19], in0=sA[:, 6:8, :, 15:19], in1=sA[:, 6:8, :, 7:11], op=ALU.add), reads=[sA], writes=[sB])
        for c in range(8):
            g = c // 2
            src = sA if g in (0, 2) else sB
            K.op(dve, lambda e: e.tensor_tensor(out=src[:, c, :, 15:19], in0=src[:, c, :, 15:19], in1=rcb[:, g, 0:NTS].rearrange("p (b t) -> p b t", t=4), op=ALU.mult),
                 reads=[src, rcb], writes=[src])
            K.op(dve, lambda e: e.tensor_tensor(out=dd[:, c, 0:NTS].rearrange("p (b t) -> p b t", t=4), in0=src[:, c, :, 15:19], in1=exs[:, c, :, 15:19], op=ALU.subtract),
                 reads=[src, exs], writes=[dd])
        pool_matmul(NTS, NTP)

    with K.phase(nf=3, nb=2):
        NKT = NTP // 128
        Kt = K.sb("Kt", [128, 5, NTP], BF16, dma=True)
        Vt = K.sb("Vt", [128, NKT, KVL], BF16, dma=True)
        wuv = K.sb("wuv", [128, 4, NH, 128], BF16, dma=True)
        dmask = K.sb("dmask", [128, 128], dma=True)
        K.dma(Kt, Kt[:, 0:4, :], KT.t[0:512, 0:NTP].rearrange("(a p) t -> p a t", p=128), writes=[Kt])
        K.dma(Kt, Kt[0:64, 4, :], KT.t[512:576, 0:NTP], writes=[Kt])
        K.dma(Vt, Vt[:], VTOK.t[0:NTP, :].rearrange("(k p) c -> p k c", p=128), writes=[Vt])
        for a_ in range(4):
            K.dma(wuv, wuv[:, a_, :, :], W["w_kv_b"][a_ * 128:(a_ + 1) * 128, :].rearrange("p (h x) -> p h x", x=256)[:, :, 128:256], writes=[wuv])
        K.dma(dmask, dmask[:], c_dmask.t, writes=[dmask])
        qT = K.sb("qT", [128, NH, 5, 128], BF16, dma=True)
        S_sb = K.sb("S_sb", [128, NTP])
        Pb = K.sb("Pb", [128, NTP], BF16)
        mx = K.sb("mx", [128, 4])
        PTs = [K.sb(f"PTs{i}", [128, 512], BF16) for i in range(2)]
        ctxb = K.sb("ctxb", [128, KVL], BF16)
        ctxT = K.sb("ctxT", [128, 4, 128], BF16)
        ao = K.sb("ao", [128, NH, 128], BF16, dma=True)
        ctxps = [K.ps(f"ctxps{i}", [128, 512]) for i in range(2)]
        QV = QAT.t.rearrange("(h r) t -> h r t", r=576)
        K.op(dve, lambda e: e.memset(ao[:], 0.0), writes=[ao])
        for s0 in range(0, NTS, 128):
            sn = min(128, NTS - s0)
            K.dma(ao, MIXT.t[1024:3072, NTP + s0:NTP + s0 + sn].rearrange("(h p) t -> p h t", p=128), ao[:, :, 0:sn], reads=[ao])
        for qb in range(NKT):
            c0, c1 = qb * 128, (qb + 1) * 128
            nk = c1
            for a_ in range(4):
                K.dma(qT, qT[:, :, a_, :], QV[:, a_ * 128:(a_ + 1) * 128, c0:c1].rearrange("h p t -> p h t"), writes=[qT])
            K.dma(qT, qT[0:64, :, 4, :], QV[:, 512:576, c0:c1].rearrange("h p t -> p h t"), writes=[qT])
            for h in range(NH):
                for kc in range(0, nk, 512):
                    w = min(512, nk - kc)
                    ps = K.psf()
                    for c in range(5):
                        pp = 128 if c < 4 else 64
                        K.op(pe, lambda e: e.matmul(ps[:, 0:w], lhsT=qT[0:pp, h, c, :], rhs=Kt[0:pp, c, kc:kc + w], start=(c == 0), stop=(c == 4)),
                             reads=[qT, Kt], writes=[ps])
                    K.op(act, lambda e: e.activation(out=S_sb[:, kc:kc + w], in_=ps[:, 0:w], func=AF.Copy, scale=SCALE), reads=[ps], writes=[S_sb])
                K.op(dve, lambda e: e.tensor_tensor(out=S_sb[:, c0:c1], in0=S_sb[:, c0:c1], in1=dmask[:], op=ALU.add), reads=[S_sb, dmask], writes=[S_sb])
                K.op(dve, lambda e: e.reduce_max(out=mx[:, 0:1], in_=S_sb[:, 0:nk], axis=AX.X), reads=[S_sb], writes=[mx])
                K.op(dve, lambda e: e.tensor_scalar(out=mx[:, 1:2], in0=mx[:, 0:1], scalar1=-1.0, scalar2=None, op0=ALU.mult), reads=[mx], writes=[mx])
                K.op(dve, lambda e: e.memset(mx[:, 2:3], 0.0), writes=[mx])
                K.op(act, lambda e: e.activation(out=Pb[:, 0:nk], in_=S_sb[:, 0:nk], func=AF.Exp, bias=mx[:, 1:2], scale=1.0, accum_out=mx[:, 2:3]),
                     reads=[S_sb, mx], writes=[Pb, mx])
                cps = ctxps[h % 2]
                for k0 in range(0, qb + 1, 4):
                    nkt = min(4, qb + 1 - k0)
                    pt = K.psb()
                    for j in range(nkt):
                        K.op(pe, lambda e: e.transpose(out=pt[:, j * 128:(j + 1) * 128], in_=Pb[:, (k0 + j) * 128:(k0 + j + 1) * 128], identity=identb[:]),
                             reads=[Pb, identb], writes=[pt])
                    pts = PTs[(k0 // 4) % 2]
                    K.op(dve, lambda e: e.tensor_copy(out=pts[:, 0:nkt * 128], in_=pt[:, 0:nkt * 128]), reads=[pt], writes=[pts])
                    for j in range(nkt):
                        kt = k0 + j
                        K.op(pe, lambda e: e.matmul(cps[:, :], lhsT=pts[:, j * 128:(j + 1) * 128], rhs=Vt[:, kt, :], start=(kt == 0), stop=(kt == qb)),
                             reads=[pts, Vt], writes=[cps])
                K.op(dve, lambda e: e.reciprocal(out=mx[:, 3:4], in_=mx[:, 2:3]), reads=[mx], writes=[mx])
                K.op(act, lambda e: e.activation(out=ctxb[:], in_=cps[:, :], func=AF.Copy, scale=mx[:, 3:4]), reads=[cps, mx], writes=[ctxb])
                pt = K.psb()
                for cc in range(4):
                    K.op(pe, lambda e: e.transpose(out=pt[:, cc * 128:(cc + 1) * 128], in_=ctxb[:, cc * 128:(cc + 1) * 128], identity=identb[:]),
                         reads=[ctxb, identb], writes=[pt])
                K.op(dve, lambda e: e.tensor_copy(out=ctxT[:].rearrange("p a q -> p (a q)"), in_=pt[:, 0:512]), reads=[pt], writes=[ctxT])
                ps = K.psf()
                for cc in range(4):
                    K.op(pe, lambda e: e.matmul(ps[:, 0:128], lhsT=wuv[:, cc, h, :], rhs=ctxT[:, cc, :], start=(cc == 0), stop=(cc == 3)),
                         reads=[wuv, ctxT], writes=[ps])
                K.op(act, lambda e: e.activation(out=ao[:, h, :], in_=ps[:, 0:128], func=AF.Copy), reads=[ps], writes=[ao])
            K.dma(ao, MIXT.t[1024:3072, c0:c1].rearrange("(h p) t -> p h t", p=128), ao[:], reads=[ao])

    with K.phase(nf=3, nb=2):
        NKS = NP * 128 + 4
        wuv = K.sb("wuv", [128, 4, NH, 128], BF16, dma=True)
        for a_ in range(4):
            K.dma(wuv, wuv[:, a_, :, :], W["w_kv_b"][a_ * 128:(a_ + 1) * 128, :].rearrange("p (h x) -> p h x", x=256)[:, :, 128:256], writes=[wuv])
        smask = K.sb("smask", [64, 4], dma=True)
        K.dma(smask, smask[:], c_smask.t, writes=[smask])
        ptb = K.sb("ptb", [128, NB * NP], I32, dma=True)
        K.dma(ptb, ptb[:], page_table.t.rearrange("b j -> (b j)").partition_broadcast(128), writes=[ptb])
        ptf = K.sb("ptf", [128, NB * NP])
        iot_i = K.sb("iot_i", [128, 1], I32)
        iot = K.sb("iot", [128, 1])
        idx = K.sb("idx", [128, NB * NP], I32)
        K.op(pool, lambda e: e.iota(iot_i[:], pattern=[[0, 1]], base=0, channel_multiplier=1), writes=[iot_i])
        K.op(dve, lambda e: e.tensor_copy(out=iot[:], in_=iot_i[:]), reads=[iot_i], writes=[iot])
        K.op(dve, lambda e: e.tensor_copy(out=ptf[:], in_=ptb[:]), reads=[ptb], writes=[ptf])
        K.op(dve, lambda e: e.tensor_scalar(out=ptf[:], in0=ptf[:], scalar1=128.0, scalar2=iot[:, 0:1], op0=ALU.mult, op1=ALU.add), reads=[ptf, iot], writes=[ptf])
        K.op(dve, lambda e: e.tensor_copy(out=idx[:], in_=ptf[:]), reads=[ptf], writes=[idx])
        kv = K.sb("kv", [128, NP, 576], BF16, dma=True)
        qTs = K.sb("qTs", [128, 5, 64], BF16, dma=True)
        KTg = K.sb("KTg", [128, 5, 512], BF16)
        KTn = K.sb("KTn", [128, 5, 4], BF16, dma=True)
        Vn = K.sb("Vn", [4, KVL], BF16, dma=True)
        S_s = K.sb("S_s", [64, NKS])
        P_s = K.sb("P_s", [64, NKS], BF16)
        mxs = K.sb("mxs", [64, 4])
        ptss = [K.sb(f"ptss{i}", [128, 4, 64], BF16) for i in range(2)]
        ptn = K.sb("ptn", [4, 64], BF16)
        ctxs = K.sb("ctxs", [64, KVL], BF16)
        ctxTs = K.sb("ctxTs", [128, 4, 64], BF16)
        aos = K.sb("aos", [128, NH, 4], BF16, dma=True)
        cps = K.ps("cps_s", [128, 512])
        QV = QAT.t.rearrange("(h r) t -> h r t", r=576)
        for b in range(NB):
            col0 = NTP + 4 * b
            for c in range(5):
                pp = 128 if c < 4 else 64
                K.dma(qTs, qTs[0:pp, c, :].rearrange("p (h t) -> p h t", t=4), QV[:, c * 128:c * 128 + pp, col0:col0 + 4].rearrange("h p t -> p h t"), writes=[qTs])
            K.dma(KTn, KTn[:, 0:4, :], KT.t[0:512, col0:col0 + 4].rearrange("(a p) t -> p a t", p=128), writes=[KTn])
            K.dma(KTn, KTn[0:64, 4, :], KT.t[512:576, col0:col0 + 4], writes=[KTn])
            K.dma(Vn, Vn[:], VTOK.t[col0:col0 + 4, :], writes=[Vn])
            for j in range(NP):
                for (dst, src) in ((kv[:, j, 0:512], cache_ckv.t), (kv[:, j, 512:576], cache_krope.t)):
                    K._pre(pool, [idx], [kv])
                    ins = nc.gpsimd.indirect_dma_start(out=dst, out_offset=None, in_=src,
                                                       in_offset=bass.IndirectOffsetOnAxis(ap=idx[:, b * NP + j:b * NP + j + 1], axis=0))
                    kv.dcount += 16
                    ins.then_inc(kv.sem, 16)
                    K._post((kv.sem, kv.dcount, None), [idx], [kv])
            for g0 in range(0, NP, 4):
                ng = min(4, NP - g0)
                for c in range(5):
                    pp = 128 if c < 4 else 64
                    pt = K.psb()
                    for jj in range(ng):
                        K.op(pe, lambda e: e.transpose(out=pt[0:pp, jj * 128:(jj + 1) * 128], in_=kv[:, g0 + jj, c * 128:c * 128 + pp], identity=identb[:]),
                             reads=[kv, identb], writes=[pt])
                    K.op(dve if c % 2 else act,
                         (lambda e: e.tensor_copy(out=KTg[0:pp, c, 0:ng * 128], in_=pt[0:pp, 0:ng * 128])) if c % 2 else
                         (lambda e: e.activation(out=KTg[0:pp, c, 0:ng * 128], in_=pt[0:pp, 0:ng * 128], func=AF.Copy)), reads=[pt], writes=[KTg])
                ps = K.psf()
                for c in range(5):
                    pp = 128 if c < 4 else 64
                    K.op(pe, lambda e: e.matmul(ps[0:64, 0:ng * 128], lhsT=qTs[0:pp, c, :], rhs=KTg[0:pp, c, 0:ng * 128], start=(c == 0), stop=(c == 4)),
                         reads=[qTs, KTg], writes=[ps])
                K.op(act, lambda e: e.activation(out=S_s[:, g0 * 128:(g0 + ng) * 128], in_=ps[0:64, 0:ng * 128], func=AF.Copy, scale=SCALE), reads=[ps], writes=[S_s])
            ps = K.psf()
            for c in range(5):
                pp = 128 if c < 4 else 64
                K.op(pe, lambda e: e.matmul(ps[0:64, 0:4], lhsT=qTs[0:pp, c, :], rhs=KTn[0:pp, c, :], start=(c == 0), stop=(c == 4)), reads=[qTs, KTn], writes=[ps])
            K.op(dve, lambda e: e.scalar_tensor_tensor(out=S_s[:, NP * 128:NKS], in0=ps[0:64, 0:4], scalar=SCALE, in1=smask[:], op0=ALU.mult, op1=ALU.add),
                 reads=[ps, smask], writes=[S_s])
            K.op(dve, lambda e: e.reduce_max(out=mxs[:, 0:1], in_=S_s[:, :], axis=AX.X), reads=[S_s], writes=[mxs])
            K.op(dve, lambda e: e.tensor_scalar(out=mxs[:, 1:2], in0=mxs[:, 0:1], scalar1=-1.0, scalar2=None, op0=ALU.mult), reads=[mxs], writes=[mxs])
            K.op(dve, lambda e: e.memset(mxs[:, 2:3], 0.0), writes=[mxs])
            K.op(act, lambda e: e.activation(out=P_s[:, :], in_=S_s[:, :], func=AF.Exp, bias=mxs[:, 1:2], scale=1.0, accum_out=mxs[:, 2:3]), reads=[S_s, mxs], writes=[P_s, mxs])
            for g0 in range(0, NP, 4):
                ng = min(4, NP - g0)
                pt = K.psb()
                for jj in range(ng):
                    K.op(pe, lambda e: e.transpose(out=pt[:, jj * 64:(jj + 1) * 64], in_=P_s[:, (g0 + jj) * 128:(g0 + jj + 1) * 128], identity=identb[0:64, 0:64]),
                         reads=[P_s, identb], writes=[pt])
                pts = ptss[(g0 // 4) % 2]
                K.op(dve, lambda e: e.tensor_copy(out=pts[:].rearrange("p a q -> p (a q)")[:, 0:ng * 64], in_=pt[:, 0:ng * 64]), reads=[pt], writes=[pts])
                for jj in range(ng):
                    K.op(pe, lambda e: e.matmul(cps[0:64, :], lhsT=pts[:, jj, :], rhs=kv[:, g0 + jj, 0:512], start=(g0 + jj == 0), stop=False), reads=[pts, kv], writes=[cps])
            pt = K.psb()
            K.op(pe, lambda e: e.transpose(out=pt[0:4, 0:64], in_=P_s[:, NP * 128:NKS], identity=identb[0:64, 0:64]), reads=[P_s, identb], writes=[pt])
            K.op(dve, lambda e: e.tensor_copy(out=ptn[:], in_=pt[0:4, 0:64]), reads=[pt], writes=[ptn])
            K.op(pe, lambda e: e.matmul(cps[0:64, :], lhsT=ptn[:], rhs=Vn[:], start=False, stop=True), reads=[ptn, Vn], writes=[cps])
            K.op(dve, lambda e: e.reciprocal(out=mxs[:, 3:4], in_=mxs[:, 2:3]), reads=[mxs], writes=[mxs])
            K.op(act, lambda e: e.activation(out=ctxs[:], in_=cps[0:64, :], func=AF.Copy, scale=mxs[:, 3:4]), reads=[cps, mxs], writes=[ctxs])
            pt = K.psb()
            for cc in range(4):
                K.op(pe, lambda e: e.transpose(out=pt[:, cc * 64:(cc + 1) * 64], in_=ctxs[:, cc * 128:(cc + 1) * 128], identity=identb[0:64, 0:64]),
                     reads=[ctxs, identb], writes=[pt])
            K.op(dve, lambda e: e.tensor_copy(out=ctxTs[:].rearrange("p a q -> p (a q)"), in_=pt[:, 0:256]), reads=[pt], writes=[ctxTs])
            ps = K.psf()
            for h in range(NH):
                for cc in range(4):
                    K.op(pe, lambda e: e.matmul(ps[:, h * 4:(h + 1) * 4], lhsT=wuv[:, cc, h, :], rhs=ctxTs[:, cc, h * 4:(h + 1) * 4], start=(cc == 0), stop=(cc == 3)),
                         reads=[wuv, ctxTs], writes=[ps])
            K.op(act, lambda e: e.activation(out=aos[:].rearrange("p h t -> p (h t)"), in_=ps[:, 0:64], func=AF.Copy), reads=[ps], writes=[aos])
            K.dma(aos, MIXT.t[1024:3072, col0:col0 + 4].rearrange("(h p) t -> p h t", p=128), aos[:], reads=[aos])

    with K.phase():
        T = 512
        ws_o = WStream("ws_o", 24, 256)
        mixs = K.sb("mixs", [128, 24, T], BF16, dma=True)
        hT = K.sb("hT", [128, 16, T], dma=True)
        for t0, Tn in tok_tiles(T):
            K.dma(mixs, mixs[:, :, 0:Tn], MIXT.t[:, t0:t0 + Tn].rearrange("(a p) t -> p a t", p=128), writes=[mixs])
            K.dma(hT, hT[:, :, 0:Tn], HT.t[:, t0:t0 + Tn].rearrange("(a p) t -> p a t", p=128), writes=[hT])

            def evac_o(j, ps, wdt):
                K.op(dve, lambda e: e.tensor_tensor(out=hT[:, j, 0:Tn], in0=hT[:, j, 0:Tn], in1=ps[:, 0:Tn], op=ALU.add), reads=[hT, ps], writes=[hT])
            linear_T(mixs, 24, Tn, W["w_out_e"], 0, D, ws_o, evac_o)
            K.dma(hT, HT.t[:, t0:t0 + Tn].rearrange("(a p) t -> p a t", p=128), hT[:, :, 0:Tn], reads=[hT])

    with K.phase():
        T = 512
        nfe = col_param("norm_ffn_e", D)
        ws_gu = WStream("ws_gu", 16, 256)
        ws_d = WStream("ws_d", FC, 256)
        hT = K.sb("hT", [128, 16, T], dma=True)
        xn = K.sb("xn", [128, 16, T], BF16)
        hid = K.sb("hid", [128, FC, T], BF16)
        sq = K.sb("sq", [128, 512])
        for t0, Tn in tok_tiles(T):
            K.dma(hT, hT[:, :, 0:Tn], HT.t[:, t0:t0 + Tn].rearrange("(a p) t -> p a t", p=128), writes=[hT])
            rmsnorm_T(hT, 16, Tn, nfe, xn, D, sq)

            def evac_g(j, ps, wdt):
                K.op(act, lambda e: e.activation(out=hid[:, j, 0:Tn], in_=ps[:, 0:Tn], func=AF.Silu), reads=[ps], writes=[hid])

            def evac_u(j, ps, wdt):
                K.op(dve, lambda e: e.tensor_tensor(out=hid[:, j, 0:Tn], in0=hid[:, j, 0:Tn], in1=ps[:, 0:Tn], op=ALU.mult), reads=[hid, ps], writes=[hid])

            def evac_d(j, ps, wdt):
                K.op(dve, lambda e: e.tensor_tensor(out=hT[:, j, 0:Tn], in0=hT[:, j, 0:Tn], in1=ps[:, 0:Tn], op=ALU.add), reads=[hT, ps], writes=[hT])
            linear_T(xn, 16, Tn, W["w_ffn_gu"], 0, DFF, ws_gu, evac_g)
            linear_T(xn, 16, Tn, W["w_ffn_gu"], DFF, DFF, ws_gu, evac_u)
            linear_T(hid, FC, Tn, W["w_ffn_down"], 0, D, ws_d, evac_d)
            K.dma(hT, HT.t[:, t0:t0 + Tn].rearrange("(a p) t -> p a t", p=128), hT[:, :, 0:Tn], reads=[hT])

    with K.phase():
        T = 512
        nmo = col_param("norm_mix_o", D)
        lbp = col_param("hg_lower_bound", 2 * D)
        lb = K.sb("lb", [128, 16])
        oml = K.sb("oml", [128, 16])
        K.op(dve, lambda e: e.tensor_tensor(out=lb[:], in0=lbp[:, 16:32], in1=lbp[:, 0:16], op=ALU.subtract), reads=[lbp], writes=[lb])
        K.op(act, lambda e: e.activation(out=lb[:], in_=lb[:], func=AF.Sigmoid), reads=[lb], writes=[lb])
        K.op(dve, lambda e: e.tensor_scalar(out=oml[:], in0=lb[:], scalar1=-1.0, scalar2=1.0, op0=ALU.mult, op1=ALU.add), reads=[lb], writes=[oml])
        ws_i = WStream("ws_i", 16, 256)
        hT = K.sb("hT", [128, 16, T], dma=True)
        xn = K.sb("xn", [128, 16, T], BF16)
        sq = K.sb("sq", [128, 512])
        stg = [K.sb(f"stg{i}", [128, T], dma=True) for i in range(4)]
        stk = K.sb("stk", [128, T], dma=True)
        cnt = [0]
        for t0, Tn in tok_tiles(T):
            K.dma(hT, hT[:, :, 0:Tn], HT.t[:, t0:t0 + Tn].rearrange("(a p) t -> p a t", p=128), writes=[hT])
            rmsnorm_T(hT, 16, Tn, nmo, xn, D, sq)

            def evac_h(j, ps, wdt):
                st = stg[cnt[0] % 4]
                cnt[0] += 1
                sec, c = j // 16, j % 16
                if sec == 0 or sec == 3:
                    K.op(act, lambda e: e.activation(out=st[:, 0:Tn], in_=ps[:, 0:Tn], func=AF.Silu), reads=[ps], writes=[st])
                    row = (0 if sec == 0 else 4) * D + c * 128
                elif sec == 2:
                    K.op(act, lambda e: e.activation(out=st[:, 0:Tn], in_=ps[:, 0:Tn], func=AF.Copy), reads=[ps], writes=[st])
                    row = 3 * D + c * 128
                else:
                    K.op(act, lambda e: e.activation(out=st[:, 0:Tn], in_=ps[:, 0:Tn], func=AF.Sigmoid), reads=[ps], writes=[st])
                    K.op(dve, lambda e: e.tensor_scalar(out=st[:, 0:Tn], in0=st[:, 0:Tn], scalar1=oml[:, c:c + 1], scalar2=lb[:, c:c + 1], op0=ALU.mult, op1=ALU.add),
                         reads=[st, oml, lb], writes=[st])
                    K.op(dve, lambda e: e.tensor_scalar(out=stk[:, 0:Tn], in0=st[:, 0:Tn], scalar1=-1.0, scalar2=1.0, op0=ALU.mult, op1=ALU.add),
                         reads=[st], writes=[stk])
                    K.dma(stk, HG.t[1 * D + c * 128:1 * D + (c + 1) * 128, t0:t0 + Tn], stk[:, 0:Tn], reads=[stk])
                    K.op(act, lambda e: e.activation(out=st[:, 0:Tn], in_=st[:, 0:Tn], func=AF.Ln), reads=[st], writes=[st])
                    row = 2 * D + c * 128
                K.dma(st, HG.t[row:row + 128, t0:t0 + Tn], st[:, 0:Tn], reads=[st])
            linear_T(xn, 16, Tn, W["w_in_o"], 0, 8192, ws_i, evac_h)

    with K.phase(nf=7, nb=1):
        CP = 32
        BLK = 256
        gmask = K.sb("gmask", [32, 32], dma=True)
        K.dma(gmask, gmask[:], c_gmask.t, writes=[gmask])
        S = K.sb("S", [128, NH, 128], dma=True)
        inb = [K.sb(f"gin{i}", [128, NH, BLK], dma=True) for i in range(4)]
        otb = K.sb("otb", [128, NH, BLK], dma=True)
        bA = K.sb("bA", [128, NH, CP])
        bB = K.sb("bB", [128, NH, CP])
        eb = K.sb("eb", [128, NH, CP])
        qe = K.sb("qe", [128, NH, CP])
        kd = K.sb("kd", [128, NH, CP])
        kl = K.sb("kl", [128, NH, CP])
        atms = [K.sb(f"atm{i}", [32, 32]) for i in range(4)]
        vtks = [K.sb(f"vtk{i}", [32, 128]) for i in range(4)]
        klTs = [K.sb(f"klT{i}", [32, 128]) for i in range(4)]

        def gla_chunk(c0, C):
            lf = inb[2]
            src, dst = lf, bA
            first = True
            s_ = 1
            while s_ < C:
                sa = (lambda t: t[:, :, c0:c0 + C]) if first else (lambda t: t[:, :, 0:C])
                sv = sa(src)
                off = c0 if first else 0
                K.op(dve, lambda e: e.tensor_tensor(out=dst[:, :, s_:C], in0=src[:, :, off + s_:off + C], in1=src[:, :, off:off + C - s_], op=ALU.add),
                     reads=[src], writes=[dst])
                K.op(pool, lambda e: e.tensor_copy(out=dst[:, :, 0:s_], in_=src[:, :, off:off + s_]), reads=[src], writes=[dst])
                src, dst = dst, (bB if dst is bA else bA)
                first = False
                s_ *= 2
            bb = src
            boff = c0 if first else 0
            K.op(act, lambda e: e.activation(out=eb[:, :, 0:C], in_=bb[:, :, boff:boff + C], func=AF.Exp), reads=[bb], writes=[eb])
            K.op(dve, lambda e: e.tensor_tensor(out=qe[:, :, 0:C], in0=inb[0][:, :, c0:c0 + C], in1=eb[:, :, 0:C], op=ALU.mult), reads=[inb[0], eb], writes=[qe])
            K.op(act, lambda e: e.activation(out=kd[:, :, 0:C], in_=bb[:, :, boff:boff + C], func=AF.Exp, scale=-1.0), reads=[bb], writes=[kd])
            K.op(dve, lambda e: e.tensor_tensor(out=kd[:, :, 0:C], in0=kd[:, :, 0:C], in1=inb[1][:, :, c0:c0 + C], op=ALU.mult), reads=[kd, inb[1]], writes=[kd])
            K.op(dve, lambda e: e.tensor_tensor(out=kl[:, :, 0:C], in0=kd[:, :, 0:C], in1=eb[:, :, C - 1:C].to_broadcast([128, NH, C]), op=ALU.mult),
                 reads=[kd, eb], writes=[kl])
            for h in range(NH):
                atm, vtk, klT = atms[h % 4], vtks[h % 4], klTs[h % 4]
                pa = K.psf()
                K.op(pe, lambda e: e.matmul(pa[0:C, 0:C], lhsT=kd[:, h, 0:C], rhs=qe[:, h, 0:C], start=True, stop=True), reads=[kd, qe], writes=[pa])
                K.op(dve, lambda e: e.tensor_tensor(out=atm[0:C, 0:C], in0=pa[0:C, 0:C], in1=gmask[0:C, 0:C], op=ALU.mult), reads=[pa, gmask], writes=[atm])
                transpose_to(vtk[0:C, :], inb[3][:, h, c0:c0 + C], [inb[3]], vtk, 128, C, eng=act)
                transpose_to(klT[0:C, :], kl[:, h, 0:C], [kl], klT, 128, C, eng=act)
                po_ = K.psf()
                K.op(pe, lambda e: e.matmul(po_[:, 0:C], lhsT=S[:, h, :], rhs=qe[:, h, 0:C], start=True, stop=False), reads=[S, qe], writes=[po_])
                K.op(pe, lambda e: e.matmul(po_[:, 0:C], lhsT=vtk[0:C, :], rhs=atm[0:C, 0:C], start=False, stop=True), reads=[vtk, atm], writes=[po_])
                K.op(act, lambda e: e.activation(out=otb[:, h, c0:c0 + C], in_=po_[:, 0:C], func=AF.Copy), reads=[po_], writes=[otb])
                pn = K.psf()
                K.op(pe, lambda e: e.matmul(pn[:, 0:128], lhsT=klT[0:C, :], rhs=vtk[0:C, :], start=True, stop=True), reads=[klT, vtk], writes=[pn])
                K.op(dve, lambda e: e.scalar_tensor_tensor(out=S[:, h, :], in0=S[:, h, :], scalar=eb[:, h, C - 1:C], in1=pn[:, 0:128], op0=ALU.mult, op1=ALU.add),
                     reads=[S, eb, pn], writes=[S])

        def load_block(t0, n):
            for i, sec in enumerate((0, 1, 2, 3)):
                K.dma(inb[i], inb[i][:, :, 0:n], HG.t[sec * D:(sec + 1) * D, t0:t0 + n].rearrange("(h p) t -> p h t", p=128), writes=[inb[i]])

        K.op(dve, lambda e: e.memset(S[:], 0.0), writes=[S])
        for t0, n in tiles(0, NTP, BLK):
            load_block(t0, n)
            for c0 in range(0, n, CP):
                gla_chunk(c0, min(CP, n - c0))
            K.dma(otb, OT.t[:, t0:t0 + n].rearrange("(h p) t -> p h t", p=128), otb[:, :, 0:n], reads=[otb])
        K.dma(S, o_hgp.t.rearrange("(h k) v -> k h v", k=128), S[:], reads=[S])
        for s0, n in tiles(NTP, NT, BLK):
            load_block(s0, n)
            for bb_ in range(n // 4):
                b = (s0 - NTP) // 4 + bb_
                K.dma(S, S[:], state_hgrn.t[b * NH * 128:(b + 1) * NH * 128, :].rearrange("(h k) v -> k h v", k=128), writes=[S])
                gla_chunk(bb_ * 4, 4)
                K.dma(S, o_hgs.t[b * NH * 128:(b + 1) * NH * 128, :].rearrange("(h k) v -> k h v", k=128), S[:], reads=[S])
            K.dma(otb, OT.t[:, s0:s0 + n].rearrange("(h p) t -> p h t", p=128), otb[:, :, 0:n], reads=[otb])

    with K.phase():
        T = 512
        hgw = col_param("hg_norm", 128)
        ws_oo = WStream("ws_oo", 16, 256)
        hT = K.sb("hT", [128, 16, T], dma=True)
        oT_ = K.sb("oT_", [128, 16, T], dma=True)
        gT = K.sb("gT", [128, 16, T], dma=True)
        xo = K.sb("xo", [128, 16, T], BF16)
        sq = K.sb("sq", [128, 512])
        for t0, Tn in tok_tiles(T):
            K.dma(hT, hT[:, :, 0:Tn], HT.t[:, t0:t0 + Tn].rearrange("(a p) t -> p a t", p=128), writes=[hT])
            K.dma(oT_, oT_[:, :, 0:Tn], OT.t[:, t0:t0 + Tn].rearrange("(a p) t -> p a t", p=128), writes=[oT_])
            K.dma(gT, gT[:, :, 0:Tn], HG.t[4 * D:5 * D, t0:t0 + Tn].rearrange("(a p) t -> p a t", p=128), writes=[gT])
            for c in range(16):
                ps = K.psf()
                K.op(act, lambda e: e.activation(out=sq[:, 0:Tn], in_=oT_[:, c, 0:Tn], func=AF.Square), reads=[oT_], writes=[sq])
                K.op(pe, lambda e: e.matmul(ps[:, 0:Tn], lhsT=ones[:], rhs=sq[:, 0:Tn], start=True, stop=True), reads=[ones, sq], writes=[ps])
                K.op(dve, lambda e: e.tensor_scalar(out=sq[:, 0:Tn], in0=ps[:, 0:Tn], scalar1=1.0 / 128, scalar2=EPS, op0=ALU.mult, op1=ALU.add), reads=[ps], writes=[sq])
                K.op(act, lambda e: e.activation(out=sq[:, 0:Tn], in_=sq[:, 0:Tn], func=AF.Ln), reads=[sq], writes=[sq])
                K.op(act, lambda e: e.activation(out=sq[:, 0:Tn], in_=sq[:, 0:Tn], func=AF.Exp, scale=-0.5), reads=[sq], writes=[sq])
                K.op(dve, lambda e: e.scalar_tensor_tensor(out=oT_[:, c, 0:Tn], in0=oT_[:, c, 0:Tn], scalar=hgw[:, 0:1], in1=sq[:, 0:Tn], op0=ALU.mult, op1=ALU.mult),
                     reads=[oT_, hgw, sq], writes=[oT_])
                K.op(dve, lambda e: e.tensor_tensor(out=xo[:, c, 0:Tn], in0=oT_[:, c, 0:Tn], in1=gT[:, c, 0:Tn], op=ALU.mult), reads=[oT_, gT], writes=[xo])

            def evac_oo(j, ps, wdt):
                K.op(dve, lambda e: e.tensor_tensor(out=hT[:, j, 0:Tn], in0=hT[:, j, 0:Tn], in1=ps[:, 0:Tn], op=ALU.add), reads=[hT, ps], writes=[hT])
            linear_T(xo, 16, Tn, W["w_out_o"], 0, D, ws_oo, evac_oo)
            K.dma(hT, HT.t[:, t0:t0 + Tn].rearrange("(a p) t -> p a t", p=128), hT[:, :, 0:Tn], reads=[hT])

    with K.phase():
        T = 512
        nfo = col_param("norm_ffn_o", D)
        wr = K.sb("wr", [128, 16, NE], dma=True)
        K.dma(wr, wr[:], w_router.t.rearrange("(a p) e -> p a e", p=128), writes=[wr])
        br = K.sb("br", [NE, 1], dma=True)
        K.dma(br, br[:], small["b_router"].t.rearrange("(p o) -> p o", o=1), writes=[br])
        sel = K.sb("sel", [NE, NE * 128], dma=True)
        K.dma(sel, sel[:], c_sel.t, writes=[sel])
        ws_gu = WStream("ws_egu", 16, 256)
        ws_d = WStream("ws_ed", FCE, 256)
        hT = K.sb("hT", [128, 16, T], dma=True)
        xnf = K.sb("xnf", [128, 16, T])
        xn = K.sb("xn", [128, 16, T], BF16)
        hid = K.sb("hid", [128, FCE, T], BF16)
        tmpb = K.sb("tmpb", [128, T], BF16)
        sq = K.sb("sq", [128, 512])
        lg = K.sb("lg", [NE, T])
        lgt = K.sb("lgt", [128, NE])
        eq1 = K.sb("eq1", [128, NE])
        eq2 = K.sb("eq2", [128, NE])
        lg2 = K.sb("lg2", [128, NE])
        comb = K.sb("comb", [128, NE])
        mm = K.sb("mm", [128, 8])
        combT = K.sb("combT", [NE, T])
        cbs = K.sb("cbs", [128, NE, T])
        for t0, Tn in tok_tiles(T):
            K.dma(hT, hT[:, :, 0:Tn], HT.t[:, t0:t0 + Tn].rearrange("(a p) t -> p a t", p=128), writes=[hT])
            rmsnorm_T(hT, 16, Tn, nfo, xnf, D, sq)
            K.op(pool, lambda e: e.tensor_copy(out=xn[:, :, 0:Tn], in_=xnf[:, :, 0:Tn]), reads=[xnf], writes=[xn])
            ps = K.psf()
            for k in range(16):
                K.op(pe, lambda e: e.matmul(ps[0:NE, 0:Tn], lhsT=wr[:, k, :], rhs=xnf[:, k, 0:Tn], start=(k == 0), stop=(k == 15)), reads=[wr, xnf], writes=[ps])
            K.op(dve, lambda e: e.tensor_scalar(out=lg[:, 0:Tn], in0=ps[0:NE, 0:Tn], scalar1=br[:, 0:1], scalar2=None, op0=ALU.add), reads=[ps, br], writes=[lg])
            for s0 in range(0, Tn, 128):
                sn = min(128, Tn - s0)
                transpose_to(lgt[0:sn, :], lg[:, s0:s0 + sn], [lg], lgt, NE, sn)
                K.op(dve, lambda e: e.reduce_max(out=mm[0:sn, 0:1], in_=lgt[0:sn, :], axis=AX.X), reads=[lgt], writes=[mm])
                K.op(dve, lambda e: e.tensor_scalar(out=eq1[0:sn, :], in0=lgt[0:sn, :], scalar1=mm[0:sn, 0:1], scalar2=None, op0=ALU.is_equal), reads=[lgt, mm], writes=[eq1])
                K.op(dve, lambda e: e.scalar_tensor_tensor(out=lg2[0:sn, :], in0=eq1[0:sn, :], scalar=NEG, in1=lgt[0:sn, :], op0=ALU.mult, op1=ALU.add),
                     reads=[eq1, lgt], writes=[lg2])
                K.op(dve, lambda e: e.reduce_max(out=mm[0:sn, 1:2], in_=lg2[0:sn, :], axis=AX.X), reads=[lg2], writes=[mm])
                K.op(dve, lambda e: e.tensor_scalar(out=eq2[0:sn, :], in0=lg2[0:sn, :], scalar1=mm[0:sn, 1:2], scalar2=None, op0=ALU.is_equal), reads=[lg2, mm], writes=[eq2])
                K.op(dve, lambda e: e.tensor_tensor(out=mm[0:sn, 2:3], in0=mm[0:sn, 1:2], in1=mm[0:sn, 0:1], op=ALU.subtract), reads=[mm], writes=[mm])
                K.op(act, lambda e: e.activation(out=mm[0:sn, 3:4], in_=mm[0:sn, 2:3], func=AF.Exp), reads=[mm], writes=[mm])
                K.op(dve, lambda e: e.tensor_scalar(out=mm[0:sn, 4:5], in0=mm[0:sn, 3:4], scalar1=1.0, scalar2=None, op0=ALU.add), reads=[mm], writes=[mm])
                K.op(dve, lambda e: e.reciprocal(out=mm[0:sn, 5:6], in_=mm[0:sn, 4:5]), reads=[mm], writes=[mm])
                K.op(dve, lambda e: e.tensor_tensor(out=mm[0:sn, 6:7], in0=mm[0:sn, 3:4], in1=mm[0:sn, 5:6], op=ALU.mult), reads=[mm], writes=[mm])
                K.op(dve, lambda e: e.tensor_scalar(out=comb[0:sn, :], in0=eq1[0:sn, :], scalar1=mm[0:sn, 5:6], scalar2=None, op0=ALU.mult), reads=[eq1, mm], writes=[comb])
                K.op(dve, lambda e: e.scalar_tensor_tensor(out=comb[0:sn, :], in0=eq2[0:sn, :], scalar=mm[0:sn, 6:7], in1=comb[0:sn, :], op0=ALU.mult, op1=ALU.add),
                     reads=[eq2, mm, comb], writes=[comb])
                transpose_to(combT[:, s0:s0 + sn], comb[0:sn, :], [comb], combT, sn, NE)
            for ex in range(NE):
                ps = K.psf()
                K.op(pe, lambda e: e.matmul(ps[:, 0:Tn], lhsT=sel[:, ex * 128:(ex + 1) * 128], rhs=combT[:, 0:Tn], start=True, stop=True), reads=[sel, combT], writes=[ps])
                K.op(act, lambda e: e.activation(out=cbs[:, ex, 0:Tn], in_=ps[:, 0:Tn], func=AF.Copy), reads=[ps], writes=[cbs])
            for ex in range(NE):
                def evac_g(j, ps, wdt):
                    K.op(act, lambda e: e.activation(out=hid[:, j, 0:Tn], in_=ps[:, 0:Tn], func=AF.Silu), reads=[ps], writes=[hid])

                def evac_u(j, ps, wdt):
                    K.op(dve, lambda e: e.tensor_tensor(out=tmpb[:, 0:Tn], in0=ps[:, 0:Tn], in1=cbs[:, ex, 0:Tn], op=ALU.mult), reads=[ps, cbs], writes=[tmpb])
                    K.op(dve, lambda e: e.tensor_tensor(out=hid[:, j, 0:Tn], in0=hid[:, j, 0:Tn], in1=tmpb[:, 0:Tn], op=ALU.mult), reads=[hid, tmpb], writes=[hid])

                def evac_d(j, ps, wdt):
                    K.op(dve, lambda e: e.tensor_tensor(out=hT[:, j, 0:Tn], in0=hT[:, j, 0:Tn], in1=ps[:, 0:Tn], op=ALU.add), reads=[hT, ps], writes=[hT])
                linear_T(xn, 16, Tn, W[f"w_exp_gu_e{ex}"], 0, DFFE, ws_gu, evac_g)
                linear_T(xn, 16, Tn, W[f"w_exp_gu_e{ex}"], DFFE, DFFE, ws_gu, evac_u)
                linear_T(hid, FCE, Tn, W[f"w_exp_down_e{ex}"], 0, D, ws_d, evac_d)
            K.dma(hT, HT.t[:, t0:t0 + Tn].rearrange("(a p) t -> p a t", p=128), hT[:, :, 0:Tn], reads=[hT])

    with K.phase():
        T = 256
        fnw = col_param("final_norm", D)
        hT = K.sb("hT", [128, 16, T], dma=True)
        yn = K.sb("yn", [128, 16, T])
        sq = K.sb("sq", [128, 512])
        ytok = K.sb("ytok", [128, D], dma=True)
        for t0, Tn in tok_tiles(T):
            issamp = t0 >= NTP
            r0 = t0 - NTP if issamp else t0
            K.dma(hT, hT[:, :, 0:Tn], HT.t[:, t0:t0 + Tn].rearrange("(a p) t -> p a t", p=128), writes=[hT])
            rmsnorm_T(hT, 16, Tn, fnw, yn, D, sq)
            for s0 in range(0, Tn, 128):
                sn = min(128, Tn - s0)
                for c in range(16):
                    transpose_to(ytok[0:sn, c * 128:(c + 1) * 128], yn[:, c, s0:s0 + sn], [yn], ytok, 128, sn, eng=(act if c % 2 else dve))
                oy = o_ys if issamp else o_yp
                K.dma(ytok, oy.t[r0 + s0:r0 + s0 + sn, :], ytok[0:sn, :], reads=[ytok])
    if cfg.get("DEBUG"):
        with K.phase():
            dbg = eout("dbg_mix", [3072, NT], BF16)
            dtag = K.sb("dtag", [128, 1], dma=True)
            K.dma(dtag, dbg.t.rearrange("(a p) t -> p a t", p=128), MIXT.t.rearrange("(a p) t -> p a t", p=128))
            dbg2 = eout("dbg_ht", [D, NT])
            K.dma(dtag, dbg2.t.rearrange("(a p) t -> p a t", p=128), HT.t.rearrange("(a p) t -> p a t", p=128))
    K.final_wait()
    return nc, K, None


def _consts(SEQ, NB, NE, PAST):
    NT = SEQ + NB * 4
    pos = np.concatenate([np.arange(SEQ), np.tile(PAST + np.arange(4), NB)]).astype(np.float32)
    inv = (np.float32(10000.0) ** (-np.arange(0, 64, 2, dtype=np.float32) / np.float32(64))).astype(np.float32)
    ang = (pos[:, None] * inv[None, :]).astype(np.float32)
    cos = np.cos(ang).astype(np.float32).T
    sin = np.sin(ang).astype(np.float32).T
    c = {}
    c["c_cos"] = np.ascontiguousarray(np.concatenate([cos, cos], 0))
    c["c_sin"] = np.ascontiguousarray(np.concatenate([sin, sin], 0))
    rc = np.zeros((4, NT), np.float32)
    for g, w in enumerate((2, 4, 8, 16)):
        rc[g] = 1.0 / np.minimum(pos + 1, w)
    c["c_rc"] = rc
    c["c_ident"] = np.eye(128, dtype=np.float32)
    p = np.arange(128)
    c["c_dmask"] = np.where(p[None, :] <= p[:, None], 0.0, NEG).astype(np.float32)
    s = np.arange(32)
    c["c_gmask"] = (s[:, None] <= s[None, :]).astype(np.float32)
    r = np.arange(64)
    c["c_smask"] = np.where(np.arange(4)[None, :] <= (r % 4)[:, None], 0.0, NEG).astype(np.float32)
    pr = np.zeros((64, 64), np.float32)
    for m in range(32):
        pr[m + 32, m] = -1.0
        pr[m, m + 32] = 1.0
    c["c_prot"] = pr
    sel = np.zeros((NE, NE * 128), np.float32)
    for e in range(NE):
        sel[e, e * 128:(e + 1) * 128] = 1.0
    c["c_sel"] = sel
    return c


def _shard_rows(a2, c):
    R, C = a2.shape
    rows = R // NCORES
    return np.ascontiguousarray(a2[c * rows:(c + 1) * rows])


_CACHE = {}
_DEBUG = False
_LAST = {}


def kernel(**inp):
    x_prompt = inp["x_prompt"]
    B, SEQ, _ = x_prompt.shape
    DB = inp["x_sample"].shape[0]
    NB = DB // NCORES
    NP = inp["page_table"].shape[1]
    NPHYS = inp["cache_ckv"].shape[1]
    DFF = inp["w_ffn_down"].shape[1]
    NE, DFFE = inp["w_exp_down"].shape[1], inp["w_exp_down"].shape[2]
    cfg = dict(SEQ=SEQ, NB=NB, NP=NP, NPHYS=NPHYS, DFF=DFF, DFFE=DFFE, NE=NE)
    if _DEBUG:
        cfg["DEBUG"] = 1
    key = tuple(sorted(cfg.items()))
    if key not in _CACHE:
        _CACHE[key] = build(cfg)
    nc = _CACHE[key][0]
    f32 = lambda a: np.ascontiguousarray(a, dtype=np.float32)
    gtab, PT = _gather_table(NPHYS, DFF, DFFE, NE)
    ck = np.ascontiguousarray(inp["cache_ckv"][0].reshape(-1, KVL), dtype=np.float32)
    kr = np.ascontiguousarray(inp["cache_krope"][0].reshape(-1, RD), dtype=np.float32)
    g2 = {"w_in_e": inp["w_in_e"][0], "w_q_b": inp["w_q_b"][0], "w_kv_b": inp["w_kv_b"][0],
          "pool_w": inp["pool_w"][0].reshape(1024, 256), "w_out_e": inp["w_out_e"][0], "w_ffn_gu": inp["w_ffn_gu"][0],
          "w_ffn_down": inp["w_ffn_down"][0], "w_in_o": inp["w_in_o"][0], "w_out_o": inp["w_out_o"][0]}
    for t in range(NPHYS // PT):
        g2[f"cache_ckv_t{t}"] = ck[t * PT * 128:(t + 1) * PT * 128]
        g2[f"cache_krope_t{t}"] = kr[t * PT * 128:(t + 1) * PT * 128]
    for e in range(NE):
        g2[f"w_exp_gu_e{e}"] = inp["w_exp_gu"][0, e]
        g2[f"w_exp_down_e{e}"] = inp["w_exp_down"][0, e]
    consts = _consts(SEQ, NB, NE, NP * 128)
    in_maps = []
    for c in range(NCORES):
        m = dict(consts)
        m["x_prompt"] = f32(x_prompt[c % B])
        m["x_sample"] = f32(inp["x_sample"][c * NB:(c + 1) * NB].reshape(NB * 4, D))
        m["page_table"] = np.ascontiguousarray(inp["page_table"][c * NB:(c + 1) * NB], dtype=np.int32)
        m["state_pool"] = f32(inp["state_pool"][0, c * NB:(c + 1) * NB].reshape(NB * 15, DPOOL))
        m["state_hgrn"] = f32(inp["state_hgrn"][0, c * NB:(c + 1) * NB].reshape(NB * NH * 128, 128))
        for nm in ("norm_mix_e", "q_norm", "kv_norm", "pool_scale", "norm_ffn_e", "norm_mix_o", "hg_norm",
                   "norm_ffn_o", "b_router"):
            m[nm] = f32(inp[nm][0])
        m["hg_lower_bound"] = f32(inp["hg_lower_bound"].reshape(-1))
        m["final_norm"] = f32(inp["final_norm"])
        m["w_router"] = f32(inp["w_router"][0])
        m["cache_ckv"] = ck
        m["cache_krope"] = kr
        for nm, a2 in g2.items():
            if nm not in NEEDED:
                continue
            m[nm] = np.ascontiguousarray(a2, dtype=np.float32) if REPLICATE else _shard_rows(a2, c)
        in_maps.append(m)
    res = run_bass_kernel_spmd(nc, in_maps, core_ids=list(range(NCORES)))
    R = res.results
    _LAST["R"] = R
    cat = lambda nm, cores: np.stack([R[c][nm] for c in cores], 0)
    y_p = cat("y_prompt", range(B))
    y_s = np.concatenate([R[c]["y_sample"].reshape(NB, 4, D) for c in range(NCORES)], 0)
    ckv_p = cat("ckv_p", range(B))[None]
    kr_p = cat("krope_p", range(B))[None]
    pool_p = cat("pool_p", range(B))[None]
    hg_p = cat("hgrn_p", range(B)).reshape(B, NH, 128, 128)[None]
    ckv_s = np.concatenate([R[c]["ckv_s"].reshape(NB, 4, KVL) for c in range(NCORES)], 0)[None]
    kr_s = np.concatenate([R[c]["krope_s"].reshape(NB, 4, RD) for c in range(NCORES)], 0)[None]
    pool_s = np.concatenate([R[c]["pool_s"].reshape(NB, 15, DPOOL) for c in range(NCORES)], 0)[None]
    hg_s = np.concatenate([R[c]["hgrn_s"].reshape(NB, NH, 128, 128) for c in range(NCORES)], 0)[None]
    return (y_p, y_s, ckv_p, kr_p, pool_p, hg_p, ckv_s, kr_s, pool_s, hg_s)
```
